# Optimizing a Trainium2 kernel written in Bass

```python
import math
import jax, jax.numpy as jnp
from jax import lax
import numpy as np

D_MODEL = 1024
BATCH = 8
SEQ = 8192
DEPTH = 1

MEM_LEN = 256
HEAD_DIM = 64
N_ATTN_HEADS = 8
ROT_DIM = HEAD_DIM // 4
ROPE_THETA = 500000.0
DILATED_PATTERNS = ((128, 1), (512, 4), (2048, 16))
N_RET_HEADS = 4
RET_QK_DIM = 64
RET_V_DIM = 128
RET_CHUNK = 128
RET_THETA = 10000.0
N_MEM_HEADS = 4
MEM_HEAD_DIM = D_MODEL // N_MEM_HEADS
D_FF = 2816
EPS = 1e-6
NEG_INF = -1e30

ATTN_WIDTH = N_ATTN_HEADS * HEAD_DIM
RET_QK_WIDTH = N_RET_HEADS * RET_QK_DIM
RET_WIDTH = N_RET_HEADS * RET_V_DIM
MIX_WIDTH = ATTN_WIDTH + RET_WIDTH
IN_SPLITS = (ATTN_WIDTH, ATTN_WIDTH, ATTN_WIDTH, RET_QK_WIDTH, RET_QK_WIDTH, RET_WIDTH, RET_WIDTH)
IN_COLS = sum(IN_SPLITS)

kernel_name = "hymba_dilated_retention_macaron"


def rmsnorm(x, w):
    x32 = x.astype(jnp.float32)
    y = x32 * lax.rsqrt(jnp.mean(x32 * x32, axis=-1, keepdims=True) + EPS)
    return (y * w.astype(jnp.float32)).astype(x.dtype)


def rotary(x, pos, rot_dim, theta):
    half = rot_dim // 2
    inv = jnp.exp(-math.log(theta) * jnp.arange(half, dtype=jnp.float32) / half)
    ang = pos.astype(jnp.float32)[:, None] * inv[None, :]
    cos = jnp.cos(ang)[None, :, None, :]
    sin = jnp.sin(ang)[None, :, None, :]
    x1 = x[..., :half].astype(jnp.float32)
    x2 = x[..., half:rot_dim].astype(jnp.float32)
    rot = jnp.concatenate([x1 * cos - x2 * sin, x1 * sin + x2 * cos], axis=-1).astype(x.dtype)
    return jnp.concatenate([rot, x[..., rot_dim:]], axis=-1)


def dilated_window_attention(q, k, v, window, dilation):
    B, S, H, Dh = q.shape
    blk = window // dilation
    span = blk * dilation
    s_pad = -(-S // span) * span
    nb = s_pad // span

    def blocks(a):
        a = jnp.pad(a, ((0, 0), (0, s_pad - S), (0, 0), (0, 0)))
        return a.reshape(B, nb, blk, dilation, H, Dh)

    def with_prev(a):
        prev = jnp.pad(a, ((0, 0), (1, 0), (0, 0), (0, 0), (0, 0), (0, 0)))[:, :-1]
        return jnp.concatenate([prev, a], axis=2)

    qb = blocks(q)
    kk = with_prev(blocks(k))
    vv = with_prev(blocks(v))
    s = jnp.einsum('bnqrhd,bnkrhd->bnrhqk', qb, kk).astype(jnp.float32) * (Dh ** -0.5)
    qi = jnp.arange(blk)[:, None]
    kj = jnp.arange(2 * blk)[None, :]
    dist = blk + qi - kj
    band = (dist >= 0) & (dist <= blk)
    not_before_start = (jnp.arange(nb)[:, None, None] > 0) | (kj >= blk)[None]
    mask = band[None] & not_before_start
    s = jnp.where(mask[None, :, None, None], s, NEG_INF)
    m = jnp.max(s, axis=-1, keepdims=True)
    p = jnp.exp(s - m)
    denom = jnp.sum(p, axis=-1, keepdims=True)
    lse = (m + jnp.log(denom))[..., 0]
    o = jnp.einsum('bnrhqk,bnkrhd->bnqrhd', (p / denom).astype(v.dtype), vv)
    o = o.reshape(B, s_pad, H, Dh)[:, :S]
    lse = lse.transpose(0, 1, 4, 2, 3).reshape(B, s_pad, H)[:, :S]
    return o, lse


def mixture_of_dilations(q, k, v):
    outs, lses = [], []
    for window, dilation in DILATED_PATTERNS:
        o, lse = dilated_window_attention(q, k, v, window, dilation)
        outs.append(o)
        lses.append(lse)
    wts = jax.nn.softmax(jnp.stack(lses, axis=0), axis=0)
    o = jnp.einsum('gbsh,gbshd->bshd', wts, jnp.stack(outs, axis=0).astype(jnp.float32))
    return o.astype(q.dtype)


def retention_decays():
    return jnp.log1p(-(2.0 ** (-5.0 - jnp.arange(N_RET_HEADS, dtype=jnp.float32))))


def chunkwise_retention(q, k, v):
    B, S, H, dk = q.shape
    dv = v.shape[-1]
    C = RET_CHUNK
    N = S // C
    log_g = retention_decays()
    k = k * (dk ** -0.5)
    qc = q.reshape(B, N, C, H, dk)
    kc = k.reshape(B, N, C, H, dk)
    vc = v.reshape(B, N, C, H, dv)
    idx = jnp.arange(C, dtype=jnp.float32)
    diff = idx[:, None] - idx[None, :]
    decay_mask = jnp.where(diff[None] >= 0, jnp.exp(log_g[:, None, None] * jnp.maximum(diff, 0.0)[None]), 0.0)
    inner = jnp.einsum('bnqhd,bnkhd->bnhqk', qc, kc) * decay_mask[None, None]
    o_inner = jnp.einsum('bnhqk,bnkhe->bnqhe', inner, vc)
    k_decay = jnp.exp(log_g[:, None] * (C - 1 - idx)[None])
    kv = jnp.einsum('bnkhd,hk,bnkhe->bnhde', kc, k_decay, vc)
    chunk_decay = jnp.exp(log_g * C)[:, None, None]

    def step(R, kv_i):
        return chunk_decay * R + kv_i, R

    R0 = jnp.zeros((B, H, dk, dv), kv.dtype)
    _, R_prev = lax.scan(step, R0, jnp.moveaxis(kv, 1, 0))
    R_prev = jnp.moveaxis(R_prev, 0, 1)
    q_decay = jnp.exp(log_g[:, None] * (idx + 1.0)[None])
    o_cross = jnp.einsum('bnqhd,hq,bnhde->bnqhe', qc, q_decay, R_prev)
    return (o_inner + o_cross).reshape(B, S, H, dv)


def hybrid_mixer(h, w_in, w_out):
    B, S, _ = h.shape
    pos = jnp.arange(S)
    proj = h @ w_in
    aq, ak, av, rq, rk, rv, rg = jnp.split(proj, list(np.cumsum(IN_SPLITS)[:-1]), axis=-1)
    aq = rotary(aq.reshape(B, S, N_ATTN_HEADS, HEAD_DIM), pos, ROT_DIM, ROPE_THETA)
    ak = rotary(ak.reshape(B, S, N_ATTN_HEADS, HEAD_DIM), pos, ROT_DIM, ROPE_THETA)
    av = av.reshape(B, S, N_ATTN_HEADS, HEAD_DIM)
    o_attn = mixture_of_dilations(aq, ak, av).reshape(B, S, ATTN_WIDTH)
    rq = rotary(rq.reshape(B, S, N_RET_HEADS, RET_QK_DIM), pos, RET_QK_DIM, RET_THETA)
    rk = rotary(rk.reshape(B, S, N_RET_HEADS, RET_QK_DIM), pos, RET_QK_DIM, RET_THETA)
    rv = rv.reshape(B, S, N_RET_HEADS, RET_V_DIM)
    r = chunkwise_retention(rq, rk, rv).astype(jnp.float32)
    r = r * lax.rsqrt(jnp.mean(r * r, axis=-1, keepdims=True) + EPS)
    o_ret = (jax.nn.silu(rg.astype(jnp.float32)) * r.reshape(B, S, RET_WIDTH)).astype(h.dtype)
    return jnp.concatenate([o_attn, o_ret], axis=-1) @ w_out


def memory_cross_attention(h, mem_n, w_cq, w_ckv, w_co):
    B, S, _ = h.shape
    M = mem_n.shape[1]
    q = (h @ w_cq).reshape(B, S, N_MEM_HEADS, MEM_HEAD_DIM)
    k, v = jnp.split(mem_n @ w_ckv, 2, axis=-1)
    k = k.reshape(B, M, N_MEM_HEADS, MEM_HEAD_DIM)
    v = v.reshape(B, M, N_MEM_HEADS, MEM_HEAD_DIM)
    s = jnp.einsum('bshd,bmhd->bhsm', q, k).astype(jnp.float32) * (MEM_HEAD_DIM ** -0.5)
    p = jax.nn.softmax(s, axis=-1).astype(v.dtype)
    o = jnp.einsum('bhsm,bmhd->bshd', p, v).reshape(B, S, D_MODEL)
    return o @ w_co


def swiglu(h, w_in, w_out):
    g, u = jnp.split(h @ w_in, 2, axis=-1)
    return (jax.nn.silu(g) * u) @ w_out


def setup_inputs(seed: int = 0) -> dict:
    key = jax.random.key(seed)
    ks = jax.random.split(key, 20)

    def w(k, shape, fan_in):
        return jax.random.normal(k, shape, jnp.float32) * (fan_in ** -0.5)

    def gain(k, shape):
        return 1.0 + 0.01 * jax.random.normal(k, shape, jnp.float32)

    L = DEPTH
    return {
        "x": jax.random.normal(ks[0], (BATCH, SEQ, D_MODEL), jnp.float32),
        "mem": jax.random.normal(ks[1], (BATCH, MEM_LEN, D_MODEL), jnp.float32),
        "norm_ffn1": gain(ks[2], (L, D_MODEL)),
        "w_ffn1_in": w(ks[3], (L, D_MODEL, 2 * D_FF), D_MODEL),
        "w_ffn1_out": w(ks[4], (L, D_FF, D_MODEL), D_FF),
        "norm_mix": gain(ks[5], (L, D_MODEL)),
        "w_in": w(ks[6], (L, D_MODEL, IN_COLS), D_MODEL),
        "w_out": w(ks[7], (L, MIX_WIDTH, D_MODEL), MIX_WIDTH),
        "norm_cross": gain(ks[8], (L, D_MODEL)),
        "norm_mem": gain(ks[9], (L, D_MODEL)),
        "w_cq": w(ks[10], (L, D_MODEL, D_MODEL), D_MODEL),
        "w_ckv": w(ks[11], (L, D_MODEL, 2 * D_MODEL), D_MODEL),
        "w_co": w(ks[12], (L, D_MODEL, D_MODEL), D_MODEL),
        "norm_ffn2": gain(ks[13], (L, D_MODEL)),
        "w_ffn2_in": w(ks[14], (L, D_MODEL, 2 * D_FF), D_MODEL),
        "w_ffn2_out": w(ks[15], (L, D_FF, D_MODEL), D_FF),
        "norm_final": gain(ks[16], (D_MODEL,)),
    }


def reference(x, mem, norm_ffn1, w_ffn1_in, w_ffn1_out, norm_mix, w_in, w_out,
              norm_cross, norm_mem, w_cq, w_ckv, w_co, norm_ffn2, w_ffn2_in, w_ffn2_out,
              norm_final):
    for l in range(DEPTH):
        x = x + 0.5 * swiglu(rmsnorm(x, norm_ffn1[l]), w_ffn1_in[l], w_ffn1_out[l])
        x = x + hybrid_mixer(rmsnorm(x, norm_mix[l]), w_in[l], w_out[l])
        x = x + memory_cross_attention(rmsnorm(x, norm_cross[l]), rmsnorm(mem, norm_mem[l]),
                                       w_cq[l], w_ckv[l], w_co[l])
        x = x + 0.5 * swiglu(rmsnorm(x, norm_ffn2[l]), w_ffn2_in[l], w_ffn2_out[l])
    return rmsnorm(x, norm_final)
```

```python
import contextlib
import math
import numpy as np
import concourse.bass as bass
import concourse.mybir as mybir
from concourse.bass_utils import run_bass_kernel_spmd

F32 = mybir.dt.float32
BF16 = mybir.dt.bfloat16
AF = mybir.ActivationFunctionType
ALU = mybir.AluOpType

D = 1024
DFF = 2816
NKC = 8
MEM = 256
EPS = 1e-6
SEM_ROLL = 4000


class Buf:
    __slots__ = ("name", "writers", "readers")

    def __init__(self, name=""):
        self.name = name
        self.writers = {}
        self.readers = {}


class Op:
    __slots__ = ("eng", "fn", "deps", "signal", "ticket", "chan", "is_dma")

    def __init__(self, eng, fn, chan, is_dma):
        self.eng = eng
        self.fn = fn
        self.deps = []
        self.signal = False
        self.ticket = None
        self.chan = chan
        self.is_dma = is_dma


class Sched:
    ENGS = ("pe", "act", "dve", "pool", "sp")

    def __init__(self, nc):
        self.nc = nc
        self.ops = {e: [] for e in self.ENGS}

    def op(self, eng, fn, reads=(), writes=(), dma=None, signal=False):
        chan = ("dma", dma) if dma is not None else ("eng", eng)
        o = Op(eng, fn, chan, dma is not None)
        o.signal = signal or (dma is not None)
        deps = {}
        for b in reads:
            for w in b.writers.values():
                deps[id(w)] = w
        for b in writes:
            for w in b.writers.values():
                deps[id(w)] = w
            for r in b.readers.values():
                deps[id(r)] = r
        for d in deps.values():
            if d.chan == chan and eng == "pe" and not o.is_dma:
                continue
            d.signal = True
            o.deps.append(d)
        for b in reads:
            b.readers[chan] = o
        for b in writes:
            b.writers = {chan: o}
            b.readers = {}
        self.ops[eng].append(o)
        return o

    def emit(self, final_waits=()):
        nc = self.nc
        counts = {}
        for e in self.ENGS:
            for o in self.ops[e]:
                if o.signal:
                    counts[o.chan] = counts.get(o.chan, 0) + 1
                    o.ticket = counts[o.chan]
        sems = {}
        with contextlib.ExitStack() as es:
            for chan, cnt in counts.items():
                per = SEM_ROLL // (16 if chan[0] == "dma" else 1)
                n = (cnt + per - 1) // per
                sems[chan] = ([es.enter_context(nc.semaphore("s_%s_%s_%d" % (chan[0], chan[1], i)))
                               for i in range(n)], per)
            block = es.enter_context(nc.Block())

            def sem_of(chan, ticket):
                ss, per = sems[chan]
                i = (ticket - 1) // per
                v = ticket - i * per
                return ss[i], v * (16 if chan[0] == "dma" else 1)

            def run(engname, eng):
                waited = {}
                for o in self.ops[engname]:
                    for d in o.deps:
                        if waited.get(d.chan, 0) >= d.ticket:
                            continue
                        waited[d.chan] = d.ticket
                        s, v = sem_of(d.chan, d.ticket)
                        eng.wait_ge(s, v)
                    inst = o.fn(eng)
                    if o.signal:
                        s, v = sem_of(o.chan, o.ticket)
                        inst.then_inc(s, 16 if o.is_dma else 1)
                if engname == "sp":
                    for name in final_waits:
                        chan = ("dma", name)
                        if chan in counts:
                            s, v = sem_of(chan, counts[chan])
                            eng.wait_ge(s, v)

            @block.tensor
            def _(e):
                run("pe", e)

            @block.scalar
            def _(e):
                run("act", e)

            @block.vector
            def _(e):
                run("dve", e)

            @block.gpsimd
            def _(e):
                run("pool", e)

            @block.sync
            def _(e):
                run("sp", e)


def build(S, TT=512, NSLOT=4, stages=("ffn1", "mix", "cross", "ffn2"), CONV=True, NARROW=True):
    assert TT % 512 == 0 and S % TT == 0
    NT = S // TT
    NS = TT // 128
    NG = TT // 512
    KW = 16 + NS
    nc = bass.Bass("TRN2", target_bir_lowering=False)

    def dram(name, shape, kind="ExternalInput"):
        return nc.dram_tensor(name, list(shape), F32, kind=kind).ap()

    x_d = dram("x", [S, D])
    mem_d = dram("mem", [MEM, D])
    w1i_d = dram("w1i", [D, 2 * DFF])
    w1o_d = dram("w1o", [DFF, D])
    w2i_d = dram("w2i", [D, 2 * DFF])
    w2o_d = dram("w2o", [DFF, D])
    win_d = dram("win", [D, 4608])
    wout_d = dram("wout", [D, D])
    wcq_d = dram("wcq", [D, D])
    wckv_d = dram("wckv", [D, 2 * D])
    wco_d = dram("wco", [D, D])
    normsT_d = dram("normsT", [128, 5 * 8])
    wnf_d = dram("wnf", [128, D])
    cosA_d = dram("cosA", [128, S])
    sinA_d = dram("sinA", [128, S])
    cosR_d = dram("cosR", [128, S])
    sinR_d = dram("sinR", [128, S])
    cmask_d = dram("cmask", [128, 23 * 128])
    dT_d = dram("dT", [128, 4 * 128])
    qdec_d = dram("qdec", [128, 2 * 512])
    small_d = dram("small", [128, 8])
    ident_d = dram("ident", [128, 128])
    y_d = dram("y", [S, D], kind="ExternalOutput")

    sch = Sched(nc)

    ARENA = 105000
    arena = nc.alloc_sbuf_tensor("arena", [128, ARENA], BF16)
    apos = [0]

    def sb(n_elems, dtype=BF16, shape=None):
        w = n_elems * (2 if dtype == F32 else 1)
        a = apos[0]
        a = (a + 15) // 16 * 16
        apos[0] = a + w
        assert apos[0] <= ARENA, ("SBUF arena overflow", apos[0])
        v = arena[:, a:a + w]
        if dtype == F32:
            v = v.bitcast(F32)
        return v

    def r3(v, b):
        return v.rearrange("p (a b) -> p a b", b=b)

    psum = nc.alloc_psum_tensor("ps", [128, 8 * 512], F32)
    bankbuf = [Buf("bank%d" % i) for i in range(8)]

    def bank(i):
        return psum[:, i * 512:(i + 1) * 512]

    def bank_bf(i):
        return psum[:, i * 512:(i + 1) * 512].bitcast(BF16)

    bctr = [0]

    def nb():
        i = bctr[0] % 8
        bctr[0] += 1
        return i

    x_tm = r3(sb(NS * D, F32), D)
    x_b = [Buf("x%d" % s) for s in range(NS)]
    xnT = r3(sb(NKC * TT), TT)
    xnT_b = [Buf("xnT%d" % g) for g in range(max(NG, 1))]
    xs = [sb(D), sb(D)]
    xs_b = [Buf("xs0"), Buf("xs1")]
    junk = sb(D)
    junk_b = Buf("junk")
    stat = sb(64, F32)
    stat_b = Buf("stat")
    statcol_b = [Buf("statc%d" % i) for i in range(16)]
    wslot = [r3(sb(8 * 512), 512) for _ in range(NSLOT)]
    wslot_b = [Buf("ws%d" % i) for i in range(NSLOT)]
    normsT = r3(sb(40, F32), 8)
    wnf = sb(D, F32)
    cmask = sb(23 * 128)
    dT = r3(sb(512, F32), 128)
    qdec = r3(sb(1024, F32), 512)
    small = sb(8, F32)
    ident = sb(128)
    ones_b = sb(128)
    onesdiv = sb(128)
    const_b = Buf("const")
    KmT = r3(sb(8 * MEM), MEM)
    VmTM = r3(sb(2 * D), D)
    memkv_b = Buf("memkv")
    R32 = r3(sb(256, F32), 128)
    Rbf = r3(sb(256), 128)
    R32_b = [Buf("R32_0"), Buf("R32_1")]
    Rbf_b = [Buf("Rbf0"), Buf("Rbf1")]
    kT = r3(sb(4 * KW * 128), KW * 128)
    Vw = r3(sb(KW * 768), 768)
    kv_b = [Buf("kv%d" % i) for i in range(KW)]
    tabs = [sb(TT, F32) for _ in range(4)]
    tabs_b = [Buf("tab%d" % i) for i in range(4)]
    ftmp = [sb(512, F32) for _ in range(4)]
    ftmp_b = [Buf("ft%d" % i) for i in range(4)]
    ebuf = [sb(512) for _ in range(6)]
    ebuf_b = [Buf("e%d" % i) for i in range(6)]
    pbuf = [sb(512) for _ in range(6)]
    pbuf_b = [Buf("p%d" % i) for i in range(6)]
    ost = [sb(D, F32), sb(D, F32)]
    ost_b = [Buf("ost0"), Buf("ost1")]
    scr0 = apos[0]
    hT = r3(sb(22 * TT), TT)
    hT_b = [[Buf("h%d_%d" % (j, g)) for g in range(NG)] for j in range(22)]
    scr_end_ffn = apos[0]
    apos[0] = scr0
    qT = r3(sb(4 * TT), TT)
    rqT = r3(sb(2 * TT), TT)
    rqdT = r3(sb(2 * TT), TT)
    rkT = r3(sb(2 * TT), TT)
    rkTM = r3(sb(NS * 256), 256)
    rvTM = r3(sb(NS * 512), 512)
    rgs = r3(sb(4 * TT), TT)
    oT = r3(sb(8 * TT), TT)
    scr_end_mix = apos[0]
    apos[0] = scr0
    qcT = r3(sb(8 * TT), TT)
    ocT = r3(sb(8 * TT), TT)
    scr_end_cross = apos[0]
    apos[0] = max(scr_end_ffn, scr_end_mix, scr_end_cross)
    scr_b = Buf("scratch_stage")
    qT_b = [Buf("qT%d" % c) for c in range(4)]
    rq_b = [Buf("rq%d" % c) for c in range(2)]
    rk_b = [Buf("rk%d" % c) for c in range(2)]
    rkTM_b = [Buf("rkTM%d" % s) for s in range(NS)]
    rvTM_b = [Buf("rvTM%d" % s) for s in range(NS)]
    rgs_b = [Buf("rgs%d" % c) for c in range(4)]
    oT_b = [Buf("oT%d" % c) for c in range(8)]
    qcT_b = [Buf("qcT%d" % c) for c in range(8)]
    ocT_b = [Buf("ocT%d" % c) for c in range(8)]
    all_scr = [scr_b] + [b for r in hT_b for b in r] + qT_b + rq_b + rk_b + rkTM_b + rvTM_b + rgs_b + oT_b + qcT_b + ocT_b

    def stage_guard():
        sch.op("dve", lambda e: e.engine_nop(), writes=all_scr)

    wctr = [0]
    wbf = {}

    def conv_w(wd, K, N):
        t = nc.dram_tensor(wd.tensor.name + "_bf", [K, N], BF16, kind="Internal").ap()
        b = Buf("cv_" + wd.tensor.name)
        sch.op("pool", lambda e: e.dma_start(out=t.rearrange("(p a) n -> p (a n)", p=128),
                                             in_=wd.rearrange("(p a) n -> p (a n)", p=128)),
               writes=[b], dma="cv_" + wd.tensor.name)
        wbf[wd.tensor.name] = (t, b)

    def load_w(wd, kc0, nkc, c0, ncols=512):
        slot = wctr[0] % NSLOT
        wctr[0] += 1
        dst = wslot[slot][:, 0:nkc, 0:ncols]
        if wd.tensor.name in wbf:
            t, b = wbf[wd.tensor.name]
            src = t.rearrange("(kc p) n -> p kc n", p=128)[:, kc0:kc0 + nkc, c0:c0 + ncols]
            sch.op("sp", lambda e: e.dma_start(out=dst, in_=src), reads=[b], writes=[wslot_b[slot]], dma="w%d" % slot)
        else:
            src = wd.rearrange("(kc p) n -> p kc n", p=128)[:, kc0:kc0 + nkc, c0:c0 + ncols]
            sch.op("pool", lambda e: e.dma_start(out=dst, in_=src), writes=[wslot_b[slot]], dma="wp%d" % slot)
        return slot

    def mm_group(bk, col0, ncol, pairs, reads):
        out = bank(bk)[:, col0:col0 + ncol]
        n = len(pairs)
        for i, (l, r) in enumerate(pairs):
            sch.op("pe", lambda e, l=l, r=r, i=i: e.matmul(out, lhsT=l, rhs=r, start=(i == 0), stop=(i == n - 1)),
                   reads=reads, writes=[bankbuf[bk]])

    statc = [0]

    def rms_stats(src_ap, src_bufs):
        c = statc[0] % 16
        statc[0] += 1
        ssq = stat[:, c:c + 1]
        rstd = stat[:, 16 + c:17 + c]
        sb_ = statcol_b[c]
        sch.op("act", lambda e: e.activation(out=junk, in_=src_ap, func=AF.Square, accum_out=ssq),
               reads=src_bufs, writes=[sb_])
        sch.op("act", lambda e: e.activation(out=rstd, in_=ssq, func=AF.Sqrt, bias=EPS, scale=1.0 / D),
               reads=[sb_], writes=[sb_])
        sch.op("dve", lambda e: e.reciprocal(out=rstd, in_=rstd),
               reads=[sb_], writes=[sb_])
        return rstd, sb_

    xsc = [0]

    def norm_T(widx, ns, dst_bufs_for_s):
        for s in range(ns):
            rstd, rb = rms_stats(x_tm[:, s, :], [x_b[s]])
            k = xsc[0] % 2
            xsc[0] += 1
            sch.op("act", lambda e, s=s, k=k, rstd=rstd: e.activation(out=xs[k], in_=x_tm[:, s, :], func=AF.Copy, scale=rstd),
                   reads=[x_b[s], rb], writes=[xs_b[k]])
            bk = nb()
            pv = r3(bank_bf(bk), 128)
            for kc in range(NKC):
                sch.op("pe", lambda e, kc=kc, k=k, pv=pv: e.transpose(pv[:, kc, :], xs[k][:, kc * 128:(kc + 1) * 128], ident),
                       reads=[xs_b[k], const_b], writes=[bankbuf[bk]])
            wbc = normsT[:, widx, :].unsqueeze(2).to_broadcast([128, 8, 128])
            sch.op("dve", lambda e, s=s, pv=pv, wbc=wbc: e.tensor_tensor(out=xnT[:, :, s * 128:(s + 1) * 128], in0=pv, in1=wbc, op=ALU.mult),
                   reads=[bankbuf[bk], const_b], writes=[dst_bufs_for_s(s)])

    def ffn(wi_d, wo_d):
        stage_guard()
        fc = [0]
        for blk in range(11):
            slot = load_w(wi_d, 0, 8, blk * 512)
            for g in range(NG):
                for jj in range(2):
                    j = 2 * blk + jj
                    bg, bu = nb(), nb()
                    rhs = [xnT[:, kc, g * 512:(g + 1) * 512] for kc in range(NKC)]
                    mm_group(bg, 0, 512, [(wslot[slot][:, kc, jj * 128:(jj + 1) * 128], rhs[kc]) for kc in range(NKC)],
                             [wslot_b[slot], xnT_b[g]])
                    mm_group(bu, 0, 512, [(wslot[slot][:, kc, 256 + jj * 128:256 + (jj + 1) * 128], rhs[kc]) for kc in range(NKC)],
                             [wslot_b[slot], xnT_b[g]])
                    f = fc[0] % 4
                    fc[0] += 1
                    sch.op("act", lambda e, f=f, bg=bg: e.activation(out=ftmp[f], in_=bank(bg), func=AF.Silu),
                           reads=[bankbuf[bg]], writes=[ftmp_b[f]])
                    sch.op("dve", lambda e, f=f, bu=bu, j=j, g=g: e.tensor_tensor(out=hT[:, j, g * 512:(g + 1) * 512], in0=bank(bu), in1=ftmp[f], op=ALU.mult),
                           reads=[bankbuf[bu], ftmp_b[f]], writes=[hT_b[j][g]])
        for ch in range(2):
            for s0 in range(0, NS, 4):
                bks = [nb() for _ in range(4)]
                for blk, (kc0, nkc) in enumerate(((0, 8), (8, 8), (16, 6))):
                    slot = load_w(wo_d, kc0, nkc, ch * 512)
                    for si in range(4):
                        s = s0 + si
                        g = s // 4
                        for kc in range(kc0, kc0 + nkc):
                            sch.op("pe", lambda e, bk=bks[si], kc=kc, s=s, slot=slot, kc0=kc0: e.matmul(
                                bank(bk), lhsT=hT[:, kc, s * 128:(s + 1) * 128], rhs=wslot[slot][:, kc - kc0, :],
                                start=(kc == 0), stop=(kc == 21)),
                                reads=[wslot_b[slot]] + [hT_b[j][g] for j in range(kc0, kc0 + nkc)], writes=[bankbuf[bks[si]]])
                for si in range(4):
                    s = s0 + si
                    sch.op("dve", lambda e, s=s, ch=ch, bk=bks[si]: e.scalar_tensor_tensor(
                        out=x_tm[:, s, ch * 512:(ch + 1) * 512], in0=bank(bk), scalar=0.5,
                        in1=x_tm[:, s, ch * 512:(ch + 1) * 512], op0=ALU.mult, op1=ALU.add),
                        reads=[bankbuf[bks[si]], x_b[s]], writes=[x_b[s]])

    def out_proj(wd, srcT, src_bufs):
        slots2 = [load_w(wd, 0, 8, 0), load_w(wd, 0, 8, 512)]
        for s in range(NS):
            for ch in range(2):
                slot = slots2[ch]
                bk = nb()
                mm_group(bk, 0, 512, [(srcT[:, kc, s * 128:(s + 1) * 128], wslot[slot][:, kc, :]) for kc in range(NKC)],
                         [wslot_b[slot]] + src_bufs)
                sch.op("dve", lambda e, s=s, ch=ch, bk=bk: e.tensor_tensor(
                    out=x_tm[:, s, ch * 512:(ch + 1) * 512], in0=bank(bk),
                    in1=x_tm[:, s, ch * 512:(ch + 1) * 512], op=ALU.add),
                    reads=[bankbuf[bk], x_b[s]], writes=[x_b[s]])

    cst_loads = [(normsT.rearrange("p a b -> p (a b)"), normsT_d), (wnf, wnf_d), (dT.rearrange("p a b -> p (a b)"), dT_d),
                 (qdec.rearrange("p a b -> p (a b)"), qdec_d), (small, small_d)]
    for i, (dst, src) in enumerate(cst_loads):
        sch.op("sp", lambda e, dst=dst, src=src: e.dma_start(out=dst, in_=src), writes=[const_b], dma="cst%d" % i)
    sch.op("pool", lambda e: e.dma_start(out=cmask, in_=cmask_d), writes=[const_b], dma="cstm")
    sch.op("pool", lambda e: e.dma_start(out=ident, in_=ident_d), writes=[const_b], dma="csti")
    sch.op("dve", lambda e: e.memset(ones_b, 1.0), writes=[const_b])
    sch.op("dve", lambda e: e.memset(onesdiv, 1.0 / 128), writes=[const_b])
    sch.op("dve", lambda e: e.memset(Vw.rearrange("p s (c w) -> p (s c) w", w=192)[:, :, 64:128], 1.0), writes=kv_b)
    sch.op("dve", lambda e: e.memset(R32.rearrange("p a b -> p (a b)"), 0.0), writes=R32_b)
    sch.op("dve", lambda e: e.memset(Rbf.rearrange("p a b -> p (a b)"), 0.0), writes=Rbf_b)
    kdec = small[:, 0:4]
    cdec = small[:, 4:6]

    if CONV:
        for wd, K, N in ((w1i_d, D, 2 * DFF), (w1o_d, DFF, D), (win_d, D, 4608), (wout_d, D, D), (wcq_d, D, D),
                         (wco_d, D, D), (w2i_d, D, 2 * DFF), (w2o_d, DFF, D)):
            conv_w(wd, K, N)

    if "cross" in stages:
        sch.op("sp", lambda e: e.dma_start(out=x_tm[:, 0:2, :], in_=mem_d.rearrange("(s p) d -> p s d", p=128)),
               writes=[x_b[0], x_b[1]], dma="xldm")
        norm_T(4, 2, lambda s: xnT_b[0])
        for blk in range(2):
            slot = load_w(wckv_d, 0, 8, blk * 512)
            for cc in range(4):
                bk = nb()
                mm_group(bk, 0, MEM, [(wslot[slot][:, kc, cc * 128:(cc + 1) * 128], xnT[:, kc, 0:MEM]) for kc in range(NKC)],
                         [wslot_b[slot], xnT_b[0]])
                sch.op("act", lambda e, bk=bk, c=blk * 4 + cc: e.activation(out=KmT[:, c, :], in_=bank(bk)[:, 0:MEM], func=AF.Copy),
                       reads=[bankbuf[bk]], writes=[memkv_b])
        for blk in range(2):
            slot = load_w(wckv_d, 0, 8, D + blk * 512)
            for m in range(2):
                bk = nb()
                mm_group(bk, 0, 512, [(xnT[:, kc, m * 128:(m + 1) * 128], wslot[slot][:, kc, :]) for kc in range(NKC)],
                         [wslot_b[slot], xnT_b[0]])
                sch.op("act", lambda e, bk=bk, m=m, blk=blk: e.activation(out=VmTM[:, m, blk * 512:(blk + 1) * 512], in_=bank(bk), func=AF.Copy),
                       reads=[bankbuf[bk]], writes=[memkv_b])

    for t in range(NT):
        t0 = t * TT
        for s in range(NS):
            sch.op("sp", lambda e, t0=t0, s=s: e.dma_start(out=x_tm[:, s, :], in_=x_d[t0 + s * 128:t0 + (s + 1) * 128, :]),
                   writes=[x_b[s]], dma="xld%d" % s)

        if "ffn1" in stages:
            norm_T(0, NS, lambda s: xnT_b[s // 4])
            ffn(w1i_d, w1o_d)

        if "mix" in stages:
            norm_T(1, NS, lambda s: xnT_b[s // 4])
            stage_guard()
            for i, td in enumerate((cosA_d, sinA_d, cosR_d, sinR_d)):
                sch.op("sp", lambda e, i=i, td=td, t0=t0: e.dma_start(out=tabs[i], in_=td[:, t0:t0 + TT]),
                       writes=[tabs_b[i]], dma="tab%d" % i)
            fc = [0]
            for blk in range(6):
                slot = load_w(win_d, 0, 8, blk * 512)
                for g in range(NG):
                    for pp in range(2):
                        ba, bb = nb(), nb()
                        rhs = [xnT[:, kc, g * 512:(g + 1) * 512] for kc in range(NKC)]
                        mm_group(ba, 0, 512, [(wslot[slot][:, kc, pp * 256:pp * 256 + 128], rhs[kc]) for kc in range(NKC)],
                                 [wslot_b[slot], xnT_b[g]])
                        mm_group(bb, 0, 512, [(wslot[slot][:, kc, pp * 256 + 128:pp * 256 + 256], rhs[kc]) for kc in range(NKC)],
                                 [wslot_b[slot], xnT_b[g]])
                        ti = 0 if blk < 4 else 2
                        cs = tabs[ti][:, g * 512:(g + 1) * 512]
                        sn = tabs[ti + 1][:, g * 512:(g + 1) * 512]
                        f1 = fc[0] % 4
                        f2 = (fc[0] + 1) % 4
                        fc[0] += 2
                        sch.op("dve", lambda e, f1=f1, ba=ba, cs=cs: e.tensor_tensor(out=ftmp[f1], in0=bank(ba), in1=cs, op=ALU.mult),
                               reads=[bankbuf[ba], tabs_b[ti]], writes=[ftmp_b[f1]])
                        sch.op("dve", lambda e, f2=f2, bb=bb, sn=sn: e.tensor_tensor(out=ftmp[f2], in0=bank(bb), in1=sn, op=ALU.mult),
                               reads=[bankbuf[bb], tabs_b[ti + 1]], writes=[ftmp_b[f2]])
                        toks = slice(g * 512, (g + 1) * 512)
                        if blk < 2:
                            c = blk * 2 + pp
                            dst, dbufs = qT[:, c, toks], [qT_b[c]]
                        elif blk < 4:
                            c = (blk - 2) * 2 + pp
                            gt = (t0 + g * 512) // 128
                            sl = gt % KW
                            dst, dbufs = kT[:, c, sl * 128:sl * 128 + 512], [kv_b[sl + i] for i in range(4)]
                        elif blk == 4:
                            dst, dbufs = rqT[:, pp, toks], [rq_b[pp]]
                        else:
                            dst, dbufs = rkT[:, pp, toks], [rk_b[pp]]
                        if blk == 4:
                            sch.op("pool", lambda e, f1=f1, f2=f2: e.tensor_tensor(out=ftmp[f1], in0=ftmp[f1], in1=ftmp[f2], op=ALU.add),
                                   reads=[ftmp_b[f1], ftmp_b[f2]], writes=[ftmp_b[f1]])
                            sch.op("pool", lambda e, f1=f1, dst=dst: e.tensor_copy(out=dst, in_=ftmp[f1]),
                                   reads=[ftmp_b[f1]], writes=dbufs)
                            sch.op("pool", lambda e, f1=f1, pp=pp, toks=toks: e.tensor_tensor(out=rqdT[:, pp, toks], in0=ftmp[f1], in1=qdec[:, pp, :], op=ALU.mult),
                                   reads=[ftmp_b[f1], const_b], writes=dbufs)
                        else:
                            sch.op("dve", lambda e, f1=f1, f2=f2, dst=dst: e.tensor_tensor(out=dst, in0=ftmp[f1], in1=ftmp[f2], op=ALU.add),
                                   reads=[ftmp_b[f1], ftmp_b[f2]], writes=dbufs)
            slot = load_w(win_d, 0, 8, 6 * 512)
            for g in range(NG):
                for cc in range(4):
                    bk = nb()
                    mm_group(bk, 0, 512, [(wslot[slot][:, kc, cc * 128:(cc + 1) * 128], xnT[:, kc, g * 512:(g + 1) * 512]) for kc in range(NKC)],
                             [wslot_b[slot], xnT_b[g]])
                    sch.op("act", lambda e, bk=bk, cc=cc, g=g: e.activation(out=rgs[:, cc, g * 512:(g + 1) * 512], in_=bank(bk), func=AF.Silu),
                           reads=[bankbuf[bk]], writes=[rgs_b[cc]])
            slot = load_w(win_d, 0, 8, 7 * 512)
            for s in range(NS):
                bk = nb()
                sl = ((t0 // 128) + s) % KW
                mm_group(bk, 0, 512, [(xnT[:, kc, s * 128:(s + 1) * 128], wslot[slot][:, kc, :]) for kc in range(NKC)],
                         [wslot_b[slot], xnT_b[s // 4]])
                for half in range(2):
                    sch.op("act", lambda e, bk=bk, sl=sl, half=half: e.activation(
                        out=Vw[:, sl, :].rearrange("p (c w) -> p c w", w=192)[:, :, half * 128:half * 128 + 64],
                        in_=bank(bk).rearrange("p (c w) -> p c w", w=128)[:, :, half * 64:(half + 1) * 64], func=AF.Copy),
                        reads=[bankbuf[bk]], writes=[kv_b[sl]])
            slot = load_w(win_d, 0, 8, 8 * 512)
            for s in range(NS):
                bk = nb()
                mm_group(bk, 0, 512, [(xnT[:, kc, s * 128:(s + 1) * 128], wslot[slot][:, kc, :]) for kc in range(NKC)],
                         [wslot_b[slot], xnT_b[s // 4]])
                sch.op("act", lambda e, bk=bk, s=s: e.activation(out=rvTM[:, s, :], in_=bank(bk), func=AF.Copy),
                       reads=[bankbuf[bk]], writes=[rvTM_b[s]])
            for s in range(NS):
                bk = nb()
                pv = r3(bank_bf(bk), 128)
                for pr in range(2):
                    sch.op("pe", lambda e, pv=pv, pr=pr, s=s: e.transpose(pv[:, pr, :], rkT[:, pr, s * 128:(s + 1) * 128], ident),
                           reads=[rk_b[pr], const_b], writes=[bankbuf[bk]])
                kb = kdec.unsqueeze(2).to_broadcast([128, 4, 64])
                sch.op("dve", lambda e, pv=pv, s=s, kb=kb: e.tensor_tensor(
                    out=rkTM[:, s, :].rearrange("p (h c) -> p h c", c=64),
                    in0=pv[:, 0:2, :].rearrange("p a (h c) -> p (a h) c", c=64), in1=kb, op=ALU.mult),
                    reads=[bankbuf[bk], const_b], writes=[rkTM_b[s]])

            ac = [0]
            for g in range(NG):
                obank = [4, 5, 6, 7]
                abank = [[0, 1], [2, 3]]

                def emit_A(s4):
                    s = g * 4 + s4
                    st = slice(s * 128, (s + 1) * 128)
                    ei = s4 % 2
                    for hh in range(2):
                        ab = abank[s4 % 2][hh]
                        ps0, ps1 = hh * 64, (hh + 1) * 64
                        for pr in range(2):
                            sch.op("pe", lambda e, ab=ab, ps0=ps0, ps1=ps1, pr=pr: e.matmul(
                                bank(ab)[:, pr * 128:(pr + 1) * 128], lhsT=rkT[ps0:ps1, pr, st], rhs=rqT[ps0:ps1, pr, st], start=True, stop=True),
                                reads=[rk_b[pr], rq_b[pr]], writes=[bankbuf[ab]])
                    for hh in range(2):
                        ab = abank[s4 % 2][hh]
                        dsel = dT.rearrange("p (pr hh) b -> p pr hh b", hh=2)[:, :, hh, :]
                        sch.op("dve", lambda e, ab=ab, hh=hh, dsel=dsel: e.tensor_tensor(
                            out=ebuf[ei][:, hh * 256:(hh + 1) * 256].rearrange("p (a b) -> p a b", b=128),
                            in0=bank(ab)[:, 0:256].rearrange("p (a b) -> p a b", b=128), in1=dsel, op=ALU.mult),
                            reads=[bankbuf[ab], const_b], writes=[ebuf_b[ei]])

                def emit_O(s4):
                    s = g * 4 + s4
                    st = slice(s * 128, (s + 1) * 128)
                    ei = s4 % 2
                    for pr in range(2):
                        for hh in range(2):
                            h = 2 * pr + hh
                            ps0, ps1 = hh * 64, (hh + 1) * 64
                            ob = obank[h]
                            ocol = bank(ob)[:, s4 * 128:(s4 + 1) * 128]
                            ecol = ebuf[ei][:, hh * 256 + pr * 128:hh * 256 + (pr + 1) * 128]
                            sch.op("pe", lambda e, ocol=ocol, h=h, ecol=ecol: e.matmul(
                                ocol, lhsT=rvTM[:, s, h * 128:(h + 1) * 128], rhs=ecol, start=True, stop=False),
                                reads=[rvTM_b[s], ebuf_b[ei]], writes=[bankbuf[ob]])
                            sch.op("pe", lambda e, ocol=ocol, ps0=ps0, ps1=ps1, pr=pr: e.matmul(
                                ocol, lhsT=Rbf[ps0:ps1, pr, :], rhs=rqdT[ps0:ps1, pr, st], start=False, stop=True),
                                reads=[Rbf_b[pr], rq_b[pr]], writes=[bankbuf[ob]])
                    for pr in range(2):
                        kb_ = abank[s4 % 2][pr]
                        sch.op("pe", lambda e, kb_=kb_, pr=pr: e.matmul(
                            bank(kb_)[:, 256:512], lhsT=rkTM[:, s, pr * 128:(pr + 1) * 128], rhs=rvTM[:, s, pr * 256:(pr + 1) * 256], start=True, stop=True),
                            reads=[rkTM_b[s], rvTM_b[s]], writes=[bankbuf[kb_]])
                        for hh in range(2):
                            ps0, ps1 = hh * 64, (hh + 1) * 64
                            sch.op("dve", lambda e, kb_=kb_, pr=pr, hh=hh, ps0=ps0, ps1=ps1: e.scalar_tensor_tensor(
                                out=R32[ps0:ps1, pr, :], in0=R32[ps0:ps1, pr, :], scalar=cdec[ps0:ps1, pr:pr + 1],
                                in1=bank(kb_)[ps0:ps1, 256 + hh * 128:256 + (hh + 1) * 128], op0=ALU.mult, op1=ALU.add),
                                reads=[bankbuf[kb_], R32_b[pr], const_b], writes=[R32_b[pr]])
                        sch.op("pool", lambda e, pr=pr: e.tensor_copy(out=Rbf[:, pr, :], in_=R32[:, pr, :]),
                               reads=[R32_b[pr]], writes=[Rbf_b[pr]])

                emit_A(0)
                for s4 in range(4):
                    if s4 + 1 < 4:
                        emit_A(s4 + 1)
                    emit_O(s4)
                ac[0] = 2
                toks = slice(g * 512, (g + 1) * 512)
                for h in range(4):
                    ob = obank[h]
                    ei = ac[0] % 3
                    ac[0] += 1
                    sch.op("act", lambda e, ei=ei, ob=ob: e.activation(out=ebuf[ei], in_=bank(ob), func=AF.Square),
                           reads=[bankbuf[ob]], writes=[ebuf_b[ei]])
                    mb = nb()
                    while mb in obank:
                        mb = nb()
                    sch.op("pe", lambda e, mb=mb, ei=ei: e.matmul(bank(mb), lhsT=onesdiv, rhs=ebuf[ei], start=True, stop=True),
                           reads=[ebuf_b[ei], const_b], writes=[bankbuf[mb]])
                    f1 = ac[0] % 4
                    f2 = (ac[0] + 1) % 4
                    sch.op("act", lambda e, f1=f1, mb=mb: e.activation(out=ftmp[f1], in_=bank(mb), func=AF.Sqrt, bias=EPS),
                           reads=[bankbuf[mb]], writes=[ftmp_b[f1]])
                    sch.op("dve", lambda e, f1=f1: e.reciprocal(out=ftmp[f1], in_=ftmp[f1]),
                           reads=[ftmp_b[f1]], writes=[ftmp_b[f1]])
                    sch.op("dve", lambda e, f1=f1, f2=f2, ob=ob: e.tensor_tensor(out=ftmp[f2], in0=bank(ob), in1=ftmp[f1], op=ALU.mult),
                           reads=[bankbuf[ob], ftmp_b[f1]], writes=[ftmp_b[f2]])
                    sch.op("pool", lambda e, f2=f2, h=h, toks=toks: e.tensor_tensor(out=oT[:, 4 + h, toks], in0=ftmp[f2], in1=rgs[:, h, toks], op=ALU.mult),
                           reads=[ftmp_b[f2], rgs_b[h]], writes=[oT_b[4 + h]])
            LA = 5
            for g in range(NG):
                Gt = (t0 + g * 512) // 512
                jall = [j for j in range(4 * Gt - 16, 4 * Gt + 4) if j >= 0]
                full = [j for j in jall if j != 4 * Gt and j >= 4 * Gt - 13 and j <= 4 * Gt]
                narrow = [j for j in jall if j != 4 * Gt and j not in full]
                if full and NARROW:
                    jlist = [(4 * Gt, 0, 4)] + [(j, max(j, 4 * Gt) - 4 * Gt, min(j + 16, 4 * Gt + 3) - 4 * Gt + 1) for j in narrow] + [(j, 0, 4) for j in full]
                else:
                    jlist = [(4 * Gt, 0, 4)] + [(j, 0, 4) for j in narrow]
                t00 = g * 512
                items = []
                accs = {}
                for c in range(4):
                    for idx, (j, q0, q1) in enumerate(jlist):
                        for hh in range(2):
                            items.append((c, hh, idx, j, q0, q1))
                SB = (0, 1, 2, 3, 4, 5)
                AB = (6, 7)

                def emit_S(i, it):
                    c, hh, idx, j, q0, q1 = it
                    ps0, ps1 = hh * 64, (hh + 1) * 64
                    sl = j % KW
                    sb_ = SB[i % 6]
                    ei = i % 6
                    c0, c1 = q0 * 128, q1 * 128
                    sch.op("pe", lambda e: e.matmul(
                        bank(sb_)[:, c0:c1], lhsT=kT[ps0:ps1, c, sl * 128:(sl + 1) * 128], rhs=qT[ps0:ps1, c, t00 + c0:t00 + c1], start=True, stop=True),
                        reads=[kv_b[sl], qT_b[c]], writes=[bankbuf[sb_]])
                    sch.op("act", lambda e: e.activation(out=ebuf[ei][:, c0:c1], in_=bank(sb_)[:, c0:c1], func=AF.Exp, scale=0.125),
                           reads=[bankbuf[sb_]], writes=[ebuf_b[ei]])
                    mo = (4 * Gt - j + 3) * 128
                    sch.op("dve" if (i % 3 != 0) else "pool", lambda e: e.tensor_tensor(
                        out=pbuf[ei][:, c0:c1], in0=ebuf[ei][:, c0:c1], in1=cmask[:, mo + c0:mo + c1], op=ALU.mult),
                        reads=[ebuf_b[ei], const_b], writes=[pbuf_b[ei]])

                def emit_PV(i, it):
                    c, hh, idx, j, q0, q1 = it
                    ps0, ps1 = hh * 64, (hh + 1) * 64
                    sl = j % KW
                    ei = i % 6
                    acc = AB[hh]
                    c0, c1 = q0 * 128, q1 * 128
                    n = len(jlist)
                    vap = Vw[:, sl, c * 192 + hh * 64:c * 192 + hh * 64 + 128]
                    sch.op("pe", lambda e: e.matmul(
                        bank(acc)[:, c0:c1], lhsT=vap, rhs=pbuf[ei][:, c0:c1], start=(idx == 0), stop=(idx == n - 1)),
                        reads=[kv_b[sl], pbuf_b[ei]], writes=[bankbuf[acc]])
                    if idx == n - 1:
                        f = (c * 2 + hh) % 4
                        d0, d1 = (1 - hh) * 64, (2 - hh) * 64
                        sch.op("dve", lambda e: e.reciprocal(out=ftmp[f][d0:d1, :], in_=bank(acc)[d0:d1, :]),
                               reads=[bankbuf[acc]], writes=[ftmp_b[f]])
                        sch.op("dve", lambda e: e.tensor_tensor(
                            out=oT[ps0:ps1, c, t00:t00 + 512], in0=bank(acc)[ps0:ps1, :], in1=ftmp[f][d0:d1, :], op=ALU.mult),
                            reads=[bankbuf[acc], ftmp_b[f]], writes=[oT_b[c]])

                for i in range(len(items) + LA):
                    if i < len(items):
                        emit_S(i, items[i])
                    if i - LA >= 0:
                        emit_PV(i - LA, items[i - LA])

            out_proj(wout_d, oT, oT_b)

        if "cross" in stages:
            norm_T(2, NS, lambda s: xnT_b[s // 4])
            stage_guard()
            for blk in range(2):
                slot = load_w(wcq_d, 0, 8, blk * 512)
                for g in range(NG):
                    for cc in range(4):
                        bk = nb()
                        c = blk * 4 + cc
                        mm_group(bk, 0, 512, [(wslot[slot][:, kc, cc * 128:(cc + 1) * 128], xnT[:, kc, g * 512:(g + 1) * 512]) for kc in range(NKC)],
                                 [wslot_b[slot], xnT_b[g]])
                        sch.op("act", lambda e, bk=bk, c=c, g=g: e.activation(out=qcT[:, c, g * 512:(g + 1) * 512], in_=bank(bk), func=AF.Copy),
                               reads=[bankbuf[bk]], writes=[qcT_b[c]])
            for g in range(NG):
                toks = slice(g * 512, (g + 1) * 512)

                def emit_CS(h):
                    for m in range(2):
                        sbk = (2 * h + m) % 4
                        ei = (2 * h + m) % 4
                        mm_group(sbk, 0, 512, [(KmT[:, 2 * h + dc, m * 128:(m + 1) * 128], qcT[:, 2 * h + dc, toks]) for dc in range(2)],
                                 [memkv_b, qcT_b[2 * h], qcT_b[2 * h + 1]])
                        sch.op("act", lambda e, ei=ei, sbk=sbk: e.activation(out=pbuf[ei], in_=bank(sbk), func=AF.Exp, scale=1.0 / 16),
                               reads=[bankbuf[sbk]], writes=[pbuf_b[ei]])

                def emit_CU(h):
                    pe_ = [(2 * h) % 4, (2 * h + 1) % 4]
                    db = 4 + (h % 2)
                    mm_group(db, 0, 512, [(ones_b, pbuf[pe_[m]]) for m in range(2)], [const_b, pbuf_b[pe_[0]], pbuf_b[pe_[1]]])
                    f = h % 4
                    sch.op("dve", lambda e, f=f, db=db: e.reciprocal(out=ftmp[f], in_=bank(db)),
                           reads=[bankbuf[db]], writes=[ftmp_b[f]])
                    for dc in range(2):
                        ub = 6 + dc
                        mm_group(ub, 0, 512, [(VmTM[:, m, (2 * h + dc) * 128:(2 * h + dc + 1) * 128], pbuf[pe_[m]]) for m in range(2)],
                                 [memkv_b, pbuf_b[pe_[0]], pbuf_b[pe_[1]]])
                        sch.op("dve", lambda e, f=f, ub=ub, c=2 * h + dc, toks=toks: e.tensor_tensor(out=ocT[:, c, toks], in0=bank(ub), in1=ftmp[f], op=ALU.mult),
                               reads=[bankbuf[ub], ftmp_b[f]], writes=[ocT_b[2 * h + dc]])

                emit_CS(0)
                for h in range(4):
                    if h + 1 < 4:
                        emit_CS(h + 1)
                    emit_CU(h)
            out_proj(wco_d, ocT, ocT_b)

        if "ffn2" in stages:
            norm_T(3, NS, lambda s: xnT_b[s // 4])
            ffn(w2i_d, w2o_d)

        for s in range(NS):
            rstd, rb = rms_stats(x_tm[:, s, :], [x_b[s]])
            k = s % 2
            sch.op("dve", lambda e, s=s, k=k, rstd=rstd: e.scalar_tensor_tensor(
                out=ost[k], in0=x_tm[:, s, :], scalar=rstd, in1=wnf, op0=ALU.mult, op1=ALU.mult),
                reads=[x_b[s], rb, const_b], writes=[ost_b[k]])
            sch.op("sp", lambda e, s=s, k=k, t0=t0: e.dma_start(out=y_d[t0 + s * 128:t0 + (s + 1) * 128, :], in_=ost[k]),
                   reads=[ost_b[k]], dma="ost%d" % k, signal=True)

    print("arena used", apos[0], "of", ARENA)
    sch.emit(final_waits=("ost0", "ost1"))
    return nc


def _consts(S):
    pos = np.arange(S, dtype=np.float32)
    half = 8
    inv = np.exp(-math.log(500000.0) * np.arange(half, dtype=np.float32) / half).astype(np.float32)
    ang = pos[None, :] * inv[:, None]
    cosA = np.ones((64, S), np.float32)
    sinA = np.zeros((64, S), np.float32)
    cosA[0:8] = np.cos(ang)
    cosA[8:16] = np.cos(ang)
    sinA[0:8] = -np.sin(ang)
    sinA[8:16] = np.sin(ang)
    cosA = np.concatenate([cosA, cosA], 0)
    sinA = np.concatenate([sinA, sinA], 0)
    half = 32
    inv = np.exp(-math.log(10000.0) * np.arange(half, dtype=np.float32) / half).astype(np.float32)
    ang = pos[None, :] * inv[:, None]
    cosR = np.concatenate([np.cos(ang), np.cos(ang)], 0)
    sinR = np.concatenate([-np.sin(ang), np.sin(ang)], 0)
    cosR = np.concatenate([cosR, cosR], 0).astype(np.float32)
    sinR = np.concatenate([sinR, sinR], 0).astype(np.float32)
    j = np.arange(128)[:, None, None]
    o = np.arange(-3, 20)[None, :, None]
    i = np.arange(128)[None, None, :]
    dl = 128 * o + i - j
    cm = ((dl >= 0) & (dl <= 128)).astype(np.float32)
    cm += ((dl >= 0) & (dl <= 512) & (dl % 4 == 0))
    cm += ((dl >= 0) & (dl <= 2048) & (dl % 16 == 0))
    cm = cm.reshape(128, 23 * 128).astype(np.float32)
    lg = np.log1p(-(2.0 ** (-5.0 - np.arange(4, dtype=np.float32)))).astype(np.float32)
    idx = np.arange(128, dtype=np.float32)
    diff = idx[None, :] - idx[:, None]
    dT = np.where(diff[None] >= 0, np.exp(lg[:, None, None] * np.maximum(diff, 0.0)[None]), 0.0) * (64 ** -0.5)
    dT = np.transpose(dT, (1, 0, 2)).reshape(128, 4 * 128).astype(np.float32)
    qd = np.exp(lg[:, None] * (idx + 1.0)[None])
    qdec = np.zeros((128, 2, 512), np.float32)
    for pr in range(2):
        for hh in range(2):
            qdec[hh * 64:(hh + 1) * 64, pr, :] = np.tile(qd[2 * pr + hh], 4)[None, :]
    qdec = qdec.reshape(128, 1024)
    small = np.zeros((128, 8), np.float32)
    kd = np.exp(lg[:, None] * (127.0 - idx)[None]) * (64 ** -0.5)
    small[:, 0:4] = kd.T
    cd = np.exp(lg * 128.0)
    for pr in range(2):
        small[0:64, 4 + pr] = cd[2 * pr]
        small[64:128, 4 + pr] = cd[2 * pr + 1]
    ident = np.eye(128, dtype=np.float32)
    return dict(cosA=cosA, sinA=sinA, cosR=cosR, sinR=sinR, cmask=cm, dT=dT, qdec=qdec, small=small, ident=ident)


def _ffn_in_layout(w):
    g = w[:, :DFF].reshape(D, 11, 256)
    u = w[:, DFF:].reshape(D, 11, 256)
    return np.ascontiguousarray(np.concatenate([g, u], axis=2).reshape(D, 2 * DFF))


def _win_layout(w):
    aq, ak, av = w[:, 0:512], w[:, 512:1024], w[:, 1024:1536]
    rq, rk = w[:, 1536:1792], w[:, 1792:2048]
    rv, rg = w[:, 2048:2560], w[:, 2560:3072]

    def partner(a, half):
        n = a.shape[1] // 64
        idx = np.arange(64)
        p = idx.copy()
        p[0:half] = idx[0:half] + half
        p[half:2 * half] = idx[half:2 * half] - half
        cols = (np.arange(n)[:, None] * 64 + p[None, :]).reshape(-1)
        return a[:, cols]

    def inter(a, ap):
        n = a.shape[1] // 128
        return np.concatenate([a.reshape(D, n, 1, 128), ap.reshape(D, n, 1, 128)], axis=2).reshape(D, 2 * a.shape[1])

    parts = [inter(aq, partner(aq, 8)), inter(ak, partner(ak, 8)), inter(rq, partner(rq, 32)),
             inter(rk, partner(rk, 32)), rg, av, rv]
    return np.ascontiguousarray(np.concatenate(parts, axis=1))


def _prep_shared(inp, S):
    f = lambda a: np.ascontiguousarray(np.asarray(a, dtype=np.float32))
    sh = dict(
        w1i=_ffn_in_layout(f(inp["w_ffn1_in"])[0]), w1o=f(inp["w_ffn1_out"])[0],
        w2i=_ffn_in_layout(f(inp["w_ffn2_in"])[0]), w2o=f(inp["w_ffn2_out"])[0],
        win=_win_layout(f(inp["w_in"])[0]), wout=f(inp["w_out"])[0],
        wcq=f(inp["w_cq"])[0], wckv=f(inp["w_ckv"])[0], wco=f(inp["w_co"])[0],
    )
    norms = np.stack([f(inp["norm_ffn1"])[0], f(inp["norm_mix"])[0], f(inp["norm_cross"])[0],
                      f(inp["norm_ffn2"])[0], f(inp["norm_mem"])[0]], 0)
    sh["normsT"] = np.ascontiguousarray(norms.reshape(5, 8, 128).transpose(2, 0, 1).reshape(128, 40))
    sh["wnf"] = np.ascontiguousarray(np.broadcast_to(f(inp["norm_final"])[None, :], (128, D)))
    sh.update(_consts(S))
    return sh


_NC_CACHE = {}


def kernel(**inputs):
    x = np.asarray(inputs["x"], dtype=np.float32)
    mem = np.asarray(inputs["mem"], dtype=np.float32)
    B, S, _ = x.shape
    sh = _prep_shared(inputs, S)
    key = (S,)
    if key not in _NC_CACHE:
        _NC_CACHE[key] = build(S)
    nc = _NC_CACHE[key]
    in_maps = []
    for b in range(B):
        m = dict(sh)
        m["x"] = np.ascontiguousarray(x[b])
        m["mem"] = np.ascontiguousarray(mem[b])
        in_maps.append(m)
    res = run_bass_kernel_spmd(nc, in_maps, core_ids=list(range(B)))
    return np.stack([np.asarray(r["y"], dtype=np.float32) for r in res.results], 0)
```

```python
import contextlib
import math
import numpy as np
import concourse.bass as bass
import concourse.mybir as mybir
from concourse.bass_utils import run_bass_kernel_spmd

F32 = mybir.dt.float32
BF16 = mybir.dt.bfloat16
AF = mybir.ActivationFunctionType
ALU = mybir.AluOpType

D = 1024
DFF = 2816
NKC = 8
MEM = 256
EPS = 1e-6
SEM_ROLL = 4000


class Buf:
    __slots__ = ("name", "writers", "readers")

    def __init__(self, name=""):
        self.name = name
        self.writers = {}
        self.readers = {}


class Op:
    __slots__ = ("eng", "fn", "deps", "signal", "ticket", "chan", "is_dma")

    def __init__(self, eng, fn, chan, is_dma):
        self.eng = eng
        self.fn = fn
        self.deps = []
        self.signal = False
        self.ticket = None
        self.chan = chan
        self.is_dma = is_dma


class Sched:
    ENGS = ("pe", "act", "dve", "pool", "sp")

    def __init__(self, nc):
        self.nc = nc
        self.ops = {e: [] for e in self.ENGS}

    def op(self, eng, fn, reads=(), writes=(), dma=None, signal=False):
        chan = ("dma", dma) if dma is not None else ("eng", eng)
        o = Op(eng, fn, chan, dma is not None)
        o.signal = signal or (dma is not None)
        deps = {}
        for b in reads:
            for w in b.writers.values():
                deps[id(w)] = w
        for b in writes:
            for w in b.writers.values():
                deps[id(w)] = w
            for r in b.readers.values():
                deps[id(r)] = r
        for d in deps.values():
            if d.chan == chan and eng == "pe" and not o.is_dma:
                continue
            d.signal = True
            o.deps.append(d)
        for b in reads:
            b.readers[chan] = o
        for b in writes:
            b.writers = {chan: o}
            b.readers = {}
        self.ops[eng].append(o)
        return o

    def emit(self, final_waits=()):
        nc = self.nc
        counts = {}
        for e in self.ENGS:
            for o in self.ops[e]:
                if o.signal:
                    counts[o.chan] = counts.get(o.chan, 0) + 1
                    o.ticket = counts[o.chan]
        sems = {}
        with contextlib.ExitStack() as es:
            for chan, cnt in counts.items():
                per = SEM_ROLL // (16 if chan[0] == "dma" else 1)
                n = (cnt + per - 1) // per
                sems[chan] = ([es.enter_context(nc.semaphore("s_%s_%s_%d" % (chan[0], chan[1], i)))
                               for i in range(n)], per)
            block = es.enter_context(nc.Block())

            def sem_of(chan, ticket):
                ss, per = sems[chan]
                i = (ticket - 1) // per
                v = ticket - i * per
                return ss[i], v * (16 if chan[0] == "dma" else 1)

            def run(engname, eng):
                waited = {}
                for o in self.ops[engname]:
                    for d in o.deps:
                        if waited.get(d.chan, 0) >= d.ticket:
                            continue
                        waited[d.chan] = d.ticket
                        s, v = sem_of(d.chan, d.ticket)
                        eng.wait_ge(s, v)
                    inst = o.fn(eng)
                    if o.signal:
                        s, v = sem_of(o.chan, o.ticket)
                        inst.then_inc(s, 16 if o.is_dma else 1)
                if engname == "sp":
                    for name in final_waits:
                        chan = ("dma", name)
                        if chan in counts:
                            s, v = sem_of(chan, counts[chan])
                            eng.wait_ge(s, v)

            @block.tensor
            def _(e):
                run("pe", e)

            @block.scalar
            def _(e):
                run("act", e)

            @block.vector
            def _(e):
                run("dve", e)

            @block.gpsimd
            def _(e):
                run("pool", e)

            @block.sync
            def _(e):
                run("sp", e)


def build(S, TT=512, NSLOT=4, stages=("ffn1", "mix", "cross", "ffn2"), CONV=True, NARROW=True):
    assert TT % 512 == 0 and S % TT == 0
    NT = S // TT
    NS = TT // 128
    NG = TT // 512
    KW = 16 + NS
    nc = bass.Bass("TRN2", target_bir_lowering=False)

    def dram(name, shape, kind="ExternalInput"):
        return nc.dram_tensor(name, list(shape), F32, kind=kind).ap()

    x_d = dram("x", [S, D])
    mem_d = dram("mem", [MEM, D])
    w1i_d = dram("w1i", [D, 2 * DFF])
    w1o_d = dram("w1o", [DFF, D])
    w2i_d = dram("w2i", [D, 2 * DFF])
    w2o_d = dram("w2o", [DFF, D])
    win_d = dram("win", [D, 4608])
    wout_d = dram("wout", [D, D])
    wcq_d = dram("wcq", [D, D])
    wckv_d = dram("wckv", [D, 2 * D])
    wco_d = dram("wco", [D, D])
    normsT_d = dram("normsT", [128, 5 * 8])
    wnf_d = dram("wnf", [128, D])
    cosA_d = dram("cosA", [128, S])
    sinA_d = dram("sinA", [128, S])
    cosR_d = dram("cosR", [128, S])
    sinR_d = dram("sinR", [128, S])
    cmask_d = dram("cmask", [128, 23 * 128])
    dT_d = dram("dT", [128, 4 * 128])
    qdec_d = dram("qdec", [128, 2 * 512])
    small_d = dram("small", [128, 8])
    ident_d = dram("ident", [128, 128])
    y_d = dram("y", [S, D], kind="ExternalOutput")

    sch = Sched(nc)

    ARENA = 105000
    arena = nc.alloc_sbuf_tensor("arena", [128, ARENA], BF16)
    apos = [0]

    def sb(n_elems, dtype=BF16, shape=None):
        w = n_elems * (2 if dtype == F32 else 1)
        a = apos[0]
        a = (a + 15) // 16 * 16
        apos[0] = a + w
        assert apos[0] <= ARENA, ("SBUF arena overflow", apos[0])
        v = arena[:, a:a + w]
        if dtype == F32:
            v = v.bitcast(F32)
        return v

    def r3(v, b):
        return v.rearrange("p (a b) -> p a b", b=b)

    psum = nc.alloc_psum_tensor("ps", [128, 8 * 512], F32)
    bankbuf = [Buf("bank%d" % i) for i in range(8)]

    def bank(i):
        return psum[:, i * 512:(i + 1) * 512]

    def bank_bf(i):
        return psum[:, i * 512:(i + 1) * 512].bitcast(BF16)

    bctr = [0]

    def nb():
        i = bctr[0] % 8
        bctr[0] += 1
        return i

    x_tm = r3(sb(NS * D, F32), D)
    x_b = [Buf("x%d" % s) for s in range(NS)]
    xnT = r3(sb(NKC * TT), TT)
    xnT_b = [Buf("xnT%d" % g) for g in range(max(NG, 1))]
    xs = [sb(D), sb(D)]
    xs_b = [Buf("xs0"), Buf("xs1")]
    junk = sb(D)
    junk_b = Buf("junk")
    stat = sb(64, F32)
    stat_b = Buf("stat")
    statcol_b = [Buf("statc%d" % i) for i in range(16)]
    wslot = [r3(sb(8 * 512), 512) for _ in range(NSLOT)]
    wslot_b = [Buf("ws%d" % i) for i in range(NSLOT)]
    normsT = r3(sb(40, F32), 8)
    wnf = sb(D, F32)
    cmask = sb(23 * 128)
    dT = r3(sb(512, F32), 128)
    qdec = r3(sb(1024, F32), 512)
    small = sb(8, F32)
    ident = sb(128)
    ones_b = sb(128)
    onesdiv = sb(128)
    const_b = Buf("const")
    KmT = r3(sb(8 * MEM), MEM)
    VmTM = r3(sb(2 * D), D)
    memkv_b = Buf("memkv")
    R32 = r3(sb(256, F32), 128)
    Rbf = r3(sb(256), 128)
    R32_b = [Buf("R32_0"), Buf("R32_1")]
    Rbf_b = [Buf("Rbf0"), Buf("Rbf1")]
    kT = r3(sb(4 * KW * 128), KW * 128)
    Vw = r3(sb(KW * 768), 768)
    kv_b = [Buf("kv%d" % i) for i in range(KW)]
    tabs = [sb(TT, F32) for _ in range(4)]
    tabs_b = [Buf("tab%d" % i) for i in range(4)]
    ftmp = [sb(512, F32) for _ in range(4)]
    ftmp_b = [Buf("ft%d" % i) for i in range(4)]
    ebuf = [sb(512) for _ in range(6)]
    ebuf_b = [Buf("e%d" % i) for i in range(6)]
    pbuf = [sb(512) for _ in range(6)]
    pbuf_b = [Buf("p%d" % i) for i in range(6)]
    ost = [sb(D, F32), sb(D, F32)]
    ost_b = [Buf("ost0"), Buf("ost1")]
    scr0 = apos[0]
    hT = r3(sb(22 * TT), TT)
    hT_b = [[Buf("h%d_%d" % (j, g)) for g in range(NG)] for j in range(22)]
    scr_end_ffn = apos[0]
    apos[0] = scr0
    qT = r3(sb(4 * TT), TT)
    rqT = r3(sb(2 * TT), TT)
    rqdT = r3(sb(2 * TT), TT)
    rkT = r3(sb(2 * TT), TT)
    rkTM = r3(sb(NS * 256), 256)
    rvTM = r3(sb(NS * 512), 512)
    rgs = r3(sb(4 * TT), TT)
    oT = r3(sb(8 * TT), TT)
    scr_end_mix = apos[0]
    apos[0] = scr0
    qcT = r3(sb(8 * TT), TT)
    ocT = r3(sb(8 * TT), TT)
    scr_end_cross = apos[0]
    apos[0] = max(scr_end_ffn, scr_end_mix, scr_end_cross)
    scr_b = Buf("scratch_stage")
    qT_b = [Buf("qT%d" % c) for c in range(4)]
    rq_b = [Buf("rq%d" % c) for c in range(2)]
    rk_b = [Buf("rk%d" % c) for c in range(2)]
    rkTM_b = [Buf("rkTM%d" % s) for s in range(NS)]
    rvTM_b = [Buf("rvTM%d" % s) for s in range(NS)]
    rgs_b = [Buf("rgs%d" % c) for c in range(4)]
    oT_b = [Buf("oT%d" % c) for c in range(8)]
    qcT_b = [Buf("qcT%d" % c) for c in range(8)]
    ocT_b = [Buf("ocT%d" % c) for c in range(8)]
    all_scr = [scr_b] + [b for r in hT_b for b in r] + qT_b + rq_b + rk_b + rkTM_b + rvTM_b + rgs_b + oT_b + qcT_b + ocT_b

    def stage_guard():
        sch.op("dve", lambda e: e.engine_nop(), writes=all_scr)

    wctr = [0]
    wbf = {}

    def conv_w(wd, K, N):
        t = nc.dram_tensor(wd.tensor.name + "_bf", [K, N], BF16, kind="Internal").ap()
        b = Buf("cv_" + wd.tensor.name)
        sch.op("pool", lambda e: e.dma_start(out=t.rearrange("(p a) n -> p (a n)", p=128),
                                             in_=wd.rearrange("(p a) n -> p (a n)", p=128)),
               writes=[b], dma="cv_" + wd.tensor.name)
        wbf[wd.tensor.name] = (t, b)

    def load_w(wd, kc0, nkc, c0, ncols=512):
        slot = wctr[0] % NSLOT
        wctr[0] += 1
        dst = wslot[slot][:, 0:nkc, 0:ncols]
        if wd.tensor.name in wbf:
            t, b = wbf[wd.tensor.name]
            src = t.rearrange("(kc p) n -> p kc n", p=128)[:, kc0:kc0 + nkc, c0:c0 + ncols]
            sch.op("sp", lambda e: e.dma_start(out=dst, in_=src), reads=[b], writes=[wslot_b[slot]], dma="w%d" % slot)
        else:
            src = wd.rearrange("(kc p) n -> p kc n", p=128)[:, kc0:kc0 + nkc, c0:c0 + ncols]
            sch.op("pool", lambda e: e.dma_start(out=dst, in_=src), writes=[wslot_b[slot]], dma="wp%d" % slot)
        return slot

    def mm_group(bk, col0, ncol, pairs, reads):
        out = bank(bk)[:, col0:col0 + ncol]
        n = len(pairs)
        for i, (l, r) in enumerate(pairs):
            sch.op("pe", lambda e, l=l, r=r, i=i: e.matmul(out, lhsT=l, rhs=r, start=(i == 0), stop=(i == n - 1)),
                   reads=reads, writes=[bankbuf[bk]])

    statc = [0]

    def rms_stats(src_ap, src_bufs):
        c = statc[0] % 16
        statc[0] += 1
        ssq = stat[:, c:c + 1]
        rstd = stat[:, 16 + c:17 + c]
        sb_ = statcol_b[c]
        sch.op("act", lambda e: e.activation(out=junk, in_=src_ap, func=AF.Square, accum_out=ssq),
               reads=src_bufs, writes=[sb_])
        sch.op("act", lambda e: e.activation(out=rstd, in_=ssq, func=AF.Sqrt, bias=EPS, scale=1.0 / D),
               reads=[sb_], writes=[sb_])
        sch.op("dve", lambda e: e.reciprocal(out=rstd, in_=rstd),
               reads=[sb_], writes=[sb_])
        return rstd, sb_

    xsc = [0]

    def norm_T(widx, ns, dst_bufs_for_s):
        for s in range(ns):
            rstd, rb = rms_stats(x_tm[:, s, :], [x_b[s]])
            k = xsc[0] % 2
            xsc[0] += 1
            sch.op("dve", lambda e, s=s, k=k, rstd=rstd: e.tensor_scalar(out=xs[k], in0=x_tm[:, s, :], scalar1=rstd, scalar2=None, op0=ALU.mult),
                   reads=[x_b[s], rb], writes=[xs_b[k]])
            bk = nb()
            pv = r3(bank_bf(bk), 128)
            for kc in range(NKC):
                sch.op("pe", lambda e, kc=kc, k=k, pv=pv: e.transpose(pv[:, kc, :], xs[k][:, kc * 128:(kc + 1) * 128], ident),
                       reads=[xs_b[k], const_b], writes=[bankbuf[bk]])
            wbc = normsT[:, widx, :].unsqueeze(2).to_broadcast([128, 8, 128])
            sch.op("dve", lambda e, s=s, pv=pv, wbc=wbc: e.tensor_tensor(out=xnT[:, :, s * 128:(s + 1) * 128], in0=pv, in1=wbc, op=ALU.mult),
                   reads=[bankbuf[bk], const_b], writes=[dst_bufs_for_s(s)])

    def ffn(wi_d, wo_d):
        stage_guard()
        fc = [0]
        for blk in range(11):
            slot = load_w(wi_d, 0, 8, blk * 512)
            for g in range(NG):
                for jj in range(2):
                    j = 2 * blk + jj
                    bg, bu = nb(), nb()
                    rhs = [xnT[:, kc, g * 512:(g + 1) * 512] for kc in range(NKC)]
                    mm_group(bg, 0, 512, [(wslot[slot][:, kc, jj * 128:(jj + 1) * 128], rhs[kc]) for kc in range(NKC)],
                             [wslot_b[slot], xnT_b[g]])
                    mm_group(bu, 0, 512, [(wslot[slot][:, kc, 256 + jj * 128:256 + (jj + 1) * 128], rhs[kc]) for kc in range(NKC)],
                             [wslot_b[slot], xnT_b[g]])
                    f = fc[0] % 4
                    fc[0] += 1
                    sch.op("act", lambda e, f=f, bg=bg: e.activation(out=ftmp[f], in_=bank(bg), func=AF.Silu),
                           reads=[bankbuf[bg]], writes=[ftmp_b[f]])
                    sch.op("dve", lambda e, f=f, bu=bu, j=j, g=g: e.tensor_tensor(out=hT[:, j, g * 512:(g + 1) * 512], in0=bank(bu), in1=ftmp[f], op=ALU.mult),
                           reads=[bankbuf[bu], ftmp_b[f]], writes=[hT_b[j][g]])
        for ch in range(2):
            for s0 in range(0, NS, 4):
                bks = [nb() for _ in range(4)]
                for blk, (kc0, nkc) in enumerate(((0, 8), (8, 8), (16, 6))):
                    slot = load_w(wo_d, kc0, nkc, ch * 512)
                    for si in range(4):
                        s = s0 + si
                        g = s // 4
                        for kc in range(kc0, kc0 + nkc):
                            sch.op("pe", lambda e, bk=bks[si], kc=kc, s=s, slot=slot, kc0=kc0: e.matmul(
                                bank(bk), lhsT=hT[:, kc, s * 128:(s + 1) * 128], rhs=wslot[slot][:, kc - kc0, :],
                                start=(kc == 0), stop=(kc == 21)),
                                reads=[wslot_b[slot]] + [hT_b[j][g] for j in range(kc0, kc0 + nkc)], writes=[bankbuf[bks[si]]])
                for si in range(4):
                    s = s0 + si
                    sch.op("dve", lambda e, s=s, ch=ch, bk=bks[si]: e.scalar_tensor_tensor(
                        out=x_tm[:, s, ch * 512:(ch + 1) * 512], in0=bank(bk), scalar=0.5,
                        in1=x_tm[:, s, ch * 512:(ch + 1) * 512], op0=ALU.mult, op1=ALU.add),
                        reads=[bankbuf[bks[si]], x_b[s]], writes=[x_b[s]])

    def out_proj(wd, srcT, src_bufs):
        slots2 = [load_w(wd, 0, 8, 0), load_w(wd, 0, 8, 512)]
        for s in range(NS):
            for ch in range(2):
                slot = slots2[ch]
                bk = nb()
                mm_group(bk, 0, 512, [(srcT[:, kc, s * 128:(s + 1) * 128], wslot[slot][:, kc, :]) for kc in range(NKC)],
                         [wslot_b[slot]] + src_bufs)
                sch.op("dve", lambda e, s=s, ch=ch, bk=bk: e.tensor_tensor(
                    out=x_tm[:, s, ch * 512:(ch + 1) * 512], in0=bank(bk),
                    in1=x_tm[:, s, ch * 512:(ch + 1) * 512], op=ALU.add),
                    reads=[bankbuf[bk], x_b[s]], writes=[x_b[s]])

    cst_loads = [(normsT.rearrange("p a b -> p (a b)"), normsT_d), (wnf, wnf_d), (dT.rearrange("p a b -> p (a b)"), dT_d),
                 (qdec.rearrange("p a b -> p (a b)"), qdec_d), (small, small_d)]
    for i, (dst, src) in enumerate(cst_loads):
        sch.op("sp", lambda e, dst=dst, src=src: e.dma_start(out=dst, in_=src), writes=[const_b], dma="cst%d" % i)
    sch.op("pool", lambda e: e.dma_start(out=cmask, in_=cmask_d), writes=[const_b], dma="cstm")
    sch.op("pool", lambda e: e.dma_start(out=ident, in_=ident_d), writes=[const_b], dma="csti")
    sch.op("dve", lambda e: e.memset(ones_b, 1.0), writes=[const_b])
    sch.op("dve", lambda e: e.memset(onesdiv, 1.0 / 128), writes=[const_b])
    sch.op("dve", lambda e: e.memset(Vw.rearrange("p s (c w) -> p (s c) w", w=192)[:, :, 64:128], 1.0), writes=kv_b)
    sch.op("dve", lambda e: e.memset(R32.rearrange("p a b -> p (a b)"), 0.0), writes=R32_b)
    sch.op("dve", lambda e: e.memset(Rbf.rearrange("p a b -> p (a b)"), 0.0), writes=Rbf_b)
    kdec = small[:, 0:4]
    cdec = small[:, 4:6]

    if CONV:
        for wd, K, N in ((w1i_d, D, 2 * DFF), (w1o_d, DFF, D), (win_d, D, 4608), (wout_d, D, D), (wcq_d, D, D),
                         (wco_d, D, D), (w2i_d, D, 2 * DFF), (w2o_d, DFF, D)):
            conv_w(wd, K, N)

    if "cross" in stages:
        sch.op("sp", lambda e: e.dma_start(out=x_tm[:, 0:2, :], in_=mem_d.rearrange("(s p) d -> p s d", p=128)),
               writes=[x_b[0], x_b[1]], dma="xldm")
        norm_T(4, 2, lambda s: xnT_b[0])
        for blk in range(2):
            slot = load_w(wckv_d, 0, 8, blk * 512)
            for cc in range(4):
                bk = nb()
                mm_group(bk, 0, MEM, [(wslot[slot][:, kc, cc * 128:(cc + 1) * 128], xnT[:, kc, 0:MEM]) for kc in range(NKC)],
                         [wslot_b[slot], xnT_b[0]])
                sch.op("act", lambda e, bk=bk, c=blk * 4 + cc: e.activation(out=KmT[:, c, :], in_=bank(bk)[:, 0:MEM], func=AF.Copy),
                       reads=[bankbuf[bk]], writes=[memkv_b])
        for blk in range(2):
            slot = load_w(wckv_d, 0, 8, D + blk * 512)
            for m in range(2):
                bk = nb()
                mm_group(bk, 0, 512, [(xnT[:, kc, m * 128:(m + 1) * 128], wslot[slot][:, kc, :]) for kc in range(NKC)],
                         [wslot_b[slot], xnT_b[0]])
                sch.op("act", lambda e, bk=bk, m=m, blk=blk: e.activation(out=VmTM[:, m, blk * 512:(blk + 1) * 512], in_=bank(bk), func=AF.Copy),
                       reads=[bankbuf[bk]], writes=[memkv_b])

    for t in range(NT):
        t0 = t * TT
        for s in range(NS):
            sch.op("sp", lambda e, t0=t0, s=s: e.dma_start(out=x_tm[:, s, :], in_=x_d[t0 + s * 128:t0 + (s + 1) * 128, :]),
                   writes=[x_b[s]], dma="xld%d" % s)

        if "ffn1" in stages:
            norm_T(0, NS, lambda s: xnT_b[s // 4])
            ffn(w1i_d, w1o_d)

        if "mix" in stages:
            norm_T(1, NS, lambda s: xnT_b[s // 4])
            stage_guard()
            for i, td in enumerate((cosA_d, sinA_d, cosR_d, sinR_d)):
                sch.op("sp", lambda e, i=i, td=td, t0=t0: e.dma_start(out=tabs[i], in_=td[:, t0:t0 + TT]),
                       writes=[tabs_b[i]], dma="tab%d" % i)
            fc = [0]
            for blk in range(6):
                slot = load_w(win_d, 0, 8, blk * 512)
                for g in range(NG):
                    for pp in range(2):
                        ba, bb = nb(), nb()
                        rhs = [xnT[:, kc, g * 512:(g + 1) * 512] for kc in range(NKC)]
                        mm_group(ba, 0, 512, [(wslot[slot][:, kc, pp * 256:pp * 256 + 128], rhs[kc]) for kc in range(NKC)],
                                 [wslot_b[slot], xnT_b[g]])
                        mm_group(bb, 0, 512, [(wslot[slot][:, kc, pp * 256 + 128:pp * 256 + 256], rhs[kc]) for kc in range(NKC)],
                                 [wslot_b[slot], xnT_b[g]])
                        ti = 0 if blk < 4 else 2
                        cs = tabs[ti][:, g * 512:(g + 1) * 512]
                        sn = tabs[ti + 1][:, g * 512:(g + 1) * 512]
                        f1 = fc[0] % 4
                        f2 = (fc[0] + 1) % 4
                        fc[0] += 2
                        sch.op("dve", lambda e, f1=f1, ba=ba, cs=cs: e.tensor_tensor(out=ftmp[f1], in0=bank(ba), in1=cs, op=ALU.mult),
                               reads=[bankbuf[ba], tabs_b[ti]], writes=[ftmp_b[f1]])
                        sch.op("dve", lambda e, f2=f2, bb=bb, sn=sn: e.tensor_tensor(out=ftmp[f2], in0=bank(bb), in1=sn, op=ALU.mult),
                               reads=[bankbuf[bb], tabs_b[ti + 1]], writes=[ftmp_b[f2]])
                        toks = slice(g * 512, (g + 1) * 512)
                        if blk < 2:
                            c = blk * 2 + pp
                            dst, dbufs = qT[:, c, toks], [qT_b[c]]
                        elif blk < 4:
                            c = (blk - 2) * 2 + pp
                            gt = (t0 + g * 512) // 128
                            sl = gt % KW
                            dst, dbufs = kT[:, c, sl * 128:sl * 128 + 512], [kv_b[sl + i] for i in range(4)]
                        elif blk == 4:
                            dst, dbufs = rqT[:, pp, toks], [rq_b[pp]]
                        else:
                            dst, dbufs = rkT[:, pp, toks], [rk_b[pp]]
                        if blk == 4:
                            sch.op("pool", lambda e, f1=f1, f2=f2: e.tensor_tensor(out=ftmp[f1], in0=ftmp[f1], in1=ftmp[f2], op=ALU.add),
                                   reads=[ftmp_b[f1], ftmp_b[f2]], writes=[ftmp_b[f1]])
                            sch.op("pool", lambda e, f1=f1, dst=dst: e.tensor_copy(out=dst, in_=ftmp[f1]),
                                   reads=[ftmp_b[f1]], writes=dbufs)
                            sch.op("pool", lambda e, f1=f1, pp=pp, toks=toks: e.tensor_tensor(out=rqdT[:, pp, toks], in0=ftmp[f1], in1=qdec[:, pp, :], op=ALU.mult),
                                   reads=[ftmp_b[f1], const_b], writes=dbufs)
                        else:
                            sch.op("dve", lambda e, f1=f1, f2=f2, dst=dst: e.tensor_tensor(out=dst, in0=ftmp[f1], in1=ftmp[f2], op=ALU.add),
                                   reads=[ftmp_b[f1], ftmp_b[f2]], writes=dbufs)
            slot = load_w(win_d, 0, 8, 6 * 512)
            for g in range(NG):
                for cc in range(4):
                    bk = nb()
                    mm_group(bk, 0, 512, [(wslot[slot][:, kc, cc * 128:(cc + 1) * 128], xnT[:, kc, g * 512:(g + 1) * 512]) for kc in range(NKC)],
                             [wslot_b[slot], xnT_b[g]])
                    sch.op("act", lambda e, bk=bk, cc=cc, g=g: e.activation(out=rgs[:, cc, g * 512:(g + 1) * 512], in_=bank(bk), func=AF.Silu),
                           reads=[bankbuf[bk]], writes=[rgs_b[cc]])
            slot = load_w(win_d, 0, 8, 7 * 512)
            for s in range(NS):
                bk = nb()
                sl = ((t0 // 128) + s) % KW
                mm_group(bk, 0, 512, [(xnT[:, kc, s * 128:(s + 1) * 128], wslot[slot][:, kc, :]) for kc in range(NKC)],
                         [wslot_b[slot], xnT_b[s // 4]])
                for half in range(2):
                    sch.op("act", lambda e, bk=bk, sl=sl, half=half: e.activation(
                        out=Vw[:, sl, :].rearrange("p (c w) -> p c w", w=192)[:, :, half * 128:half * 128 + 64],
                        in_=bank(bk).rearrange("p (c w) -> p c w", w=128)[:, :, half * 64:(half + 1) * 64], func=AF.Copy),
                        reads=[bankbuf[bk]], writes=[kv_b[sl]])
            slot = load_w(win_d, 0, 8, 8 * 512)
            for s in range(NS):
                bk = nb()
                mm_group(bk, 0, 512, [(xnT[:, kc, s * 128:(s + 1) * 128], wslot[slot][:, kc, :]) for kc in range(NKC)],
                         [wslot_b[slot], xnT_b[s // 4]])
                sch.op("act", lambda e, bk=bk, s=s: e.activation(out=rvTM[:, s, :], in_=bank(bk), func=AF.Copy),
                       reads=[bankbuf[bk]], writes=[rvTM_b[s]])
            for s in range(NS):
                bk = nb()
                pv = r3(bank_bf(bk), 128)
                for pr in range(2):
                    sch.op("pe", lambda e, pv=pv, pr=pr, s=s: e.transpose(pv[:, pr, :], rkT[:, pr, s * 128:(s + 1) * 128], ident),
                           reads=[rk_b[pr], const_b], writes=[bankbuf[bk]])
                kb = kdec.unsqueeze(2).to_broadcast([128, 4, 64])
                sch.op("dve", lambda e, pv=pv, s=s, kb=kb: e.tensor_tensor(
                    out=rkTM[:, s, :].rearrange("p (h c) -> p h c", c=64),
                    in0=pv[:, 0:2, :].rearrange("p a (h c) -> p (a h) c", c=64), in1=kb, op=ALU.mult),
                    reads=[bankbuf[bk], const_b], writes=[rkTM_b[s]])

            ac = [0]
            for g in range(NG):
                obank = [4, 5, 6, 7]
                abank = [[0, 1], [2, 3]]

                def emit_A(s4):
                    s = g * 4 + s4
                    st = slice(s * 128, (s + 1) * 128)
                    ei = s4 % 2
                    for hh in range(2):
                        ab = abank[s4 % 2][hh]
                        ps0, ps1 = hh * 64, (hh + 1) * 64
                        for pr in range(2):
                            sch.op("pe", lambda e, ab=ab, ps0=ps0, ps1=ps1, pr=pr: e.matmul(
                                bank(ab)[:, pr * 128:(pr + 1) * 128], lhsT=rkT[ps0:ps1, pr, st], rhs=rqT[ps0:ps1, pr, st], start=True, stop=True),
                                reads=[rk_b[pr], rq_b[pr]], writes=[bankbuf[ab]])
                    for hh in range(2):
                        ab = abank[s4 % 2][hh]
                        dsel = dT.rearrange("p (pr hh) b -> p pr hh b", hh=2)[:, :, hh, :]
                        sch.op("dve", lambda e, ab=ab, hh=hh, dsel=dsel: e.tensor_tensor(
                            out=ebuf[ei][:, hh * 256:(hh + 1) * 256].rearrange("p (a b) -> p a b", b=128),
                            in0=bank(ab)[:, 0:256].rearrange("p (a b) -> p a b", b=128), in1=dsel, op=ALU.mult),
                            reads=[bankbuf[ab], const_b], writes=[ebuf_b[ei]])

                def emit_O(s4):
                    s = g * 4 + s4
                    st = slice(s * 128, (s + 1) * 128)
                    ei = s4 % 2
                    for pr in range(2):
                        for hh in range(2):
                            h = 2 * pr + hh
                            ps0, ps1 = hh * 64, (hh + 1) * 64
                            ob = obank[h]
                            ocol = bank(ob)[:, s4 * 128:(s4 + 1) * 128]
                            ecol = ebuf[ei][:, hh * 256 + pr * 128:hh * 256 + (pr + 1) * 128]
                            sch.op("pe", lambda e, ocol=ocol, h=h, ecol=ecol: e.matmul(
                                ocol, lhsT=rvTM[:, s, h * 128:(h + 1) * 128], rhs=ecol, start=True, stop=False),
                                reads=[rvTM_b[s], ebuf_b[ei]], writes=[bankbuf[ob]])
                            sch.op("pe", lambda e, ocol=ocol, ps0=ps0, ps1=ps1, pr=pr: e.matmul(
                                ocol, lhsT=Rbf[ps0:ps1, pr, :], rhs=rqdT[ps0:ps1, pr, st], start=False, stop=True),
                                reads=[Rbf_b[pr], rq_b[pr]], writes=[bankbuf[ob]])
                    for pr in range(2):
                        kb_ = abank[s4 % 2][pr]
                        sch.op("pe", lambda e, kb_=kb_, pr=pr: e.matmul(
                            bank(kb_)[:, 256:512], lhsT=rkTM[:, s, pr * 128:(pr + 1) * 128], rhs=rvTM[:, s, pr * 256:(pr + 1) * 256], start=True, stop=True),
                            reads=[rkTM_b[s], rvTM_b[s]], writes=[bankbuf[kb_]])
                        for hh in range(2):
                            ps0, ps1 = hh * 64, (hh + 1) * 64
                            sch.op("dve", lambda e, kb_=kb_, pr=pr, hh=hh, ps0=ps0, ps1=ps1: e.scalar_tensor_tensor(
                                out=R32[ps0:ps1, pr, :], in0=R32[ps0:ps1, pr, :], scalar=cdec[ps0:ps1, pr:pr + 1],
                                in1=bank(kb_)[ps0:ps1, 256 + hh * 128:256 + (hh + 1) * 128], op0=ALU.mult, op1=ALU.add),
                                reads=[bankbuf[kb_], R32_b[pr], const_b], writes=[R32_b[pr]])
                        sch.op("pool", lambda e, pr=pr: e.tensor_copy(out=Rbf[:, pr, :], in_=R32[:, pr, :]),
                               reads=[R32_b[pr]], writes=[Rbf_b[pr]])

                emit_A(0)
                for s4 in range(4):
                    if s4 + 1 < 4:
                        emit_A(s4 + 1)
                    emit_O(s4)
                ac[0] = 2
                toks = slice(g * 512, (g + 1) * 512)
                mbs = [0, 1, 2, 3]
                for h in range(4):
                    sch.op("act", lambda e, h=h: e.activation(out=ebuf[2 + h], in_=bank(obank[h]), func=AF.Square),
                           reads=[bankbuf[obank[h]]], writes=[ebuf_b[2 + h]])
                for h in range(4):
                    sch.op("pe", lambda e, h=h: e.matmul(bank(mbs[h]), lhsT=onesdiv, rhs=ebuf[2 + h], start=True, stop=True),
                           reads=[ebuf_b[2 + h], const_b], writes=[bankbuf[mbs[h]]])
                for h in range(4):
                    sch.op("act", lambda e, h=h: e.activation(out=ftmp[h], in_=bank(mbs[h]), func=AF.Sqrt, bias=EPS),
                           reads=[bankbuf[mbs[h]]], writes=[ftmp_b[h]])
                for h in range(4):
                    sch.op("dve", lambda e, h=h: e.reciprocal(out=ftmp[h], in_=ftmp[h]),
                           reads=[ftmp_b[h]], writes=[ftmp_b[h]])
                    sch.op("dve", lambda e, h=h: e.tensor_tensor(out=ftmp[h], in0=bank(obank[h]), in1=ftmp[h], op=ALU.mult),
                           reads=[bankbuf[obank[h]], ftmp_b[h]], writes=[ftmp_b[h]])
                    sch.op("pool", lambda e, h=h, toks=toks: e.tensor_tensor(out=oT[:, 4 + h, toks], in0=ftmp[h], in1=rgs[:, h, toks], op=ALU.mult),
                           reads=[ftmp_b[h], rgs_b[h]], writes=[oT_b[4 + h]])
            LA = 5
            for g in range(NG):
                Gt = (t0 + g * 512) // 512
                jall = [j for j in range(4 * Gt - 16, 4 * Gt + 4) if j >= 0]
                full = [j for j in jall if j != 4 * Gt and j >= 4 * Gt - 13 and j <= 4 * Gt]
                narrow = [j for j in jall if j != 4 * Gt and j not in full]
                if full and NARROW:
                    jlist = [(4 * Gt, 0, 4)] + [(j, max(j, 4 * Gt) - 4 * Gt, min(j + 16, 4 * Gt + 3) - 4 * Gt + 1) for j in narrow] + [(j, 0, 4) for j in full]
                else:
                    jlist = [(4 * Gt, 0, 4)] + [(j, 0, 4) for j in narrow]
                t00 = g * 512
                items = []
                accs = {}
                for c in range(4):
                    for idx, (j, q0, q1) in enumerate(jlist):
                        for hh in range(2):
                            items.append((c, hh, idx, j, q0, q1))
                SB = (0, 1, 2, 3, 4, 5)
                AB = (6, 7)

                def emit_S(i, it):
                    c, hh, idx, j, q0, q1 = it
                    ps0, ps1 = hh * 64, (hh + 1) * 64
                    sl = j % KW
                    sb_ = SB[i % 6]
                    ei = i % 6
                    c0, c1 = q0 * 128, q1 * 128
                    sch.op("pe", lambda e: e.matmul(
                        bank(sb_)[:, c0:c1], lhsT=kT[ps0:ps1, c, sl * 128:(sl + 1) * 128], rhs=qT[ps0:ps1, c, t00 + c0:t00 + c1], start=True, stop=True),
                        reads=[kv_b[sl], qT_b[c]], writes=[bankbuf[sb_]])
                    sch.op("act", lambda e: e.activation(out=ebuf[ei][:, c0:c1], in_=bank(sb_)[:, c0:c1], func=AF.Exp, scale=0.125),
                           reads=[bankbuf[sb_]], writes=[ebuf_b[ei]])
                    mo = (4 * Gt - j + 3) * 128
                    sch.op("dve" if (i % 3 != 0) else "pool", lambda e: e.tensor_tensor(
                        out=pbuf[ei][:, c0:c1], in0=ebuf[ei][:, c0:c1], in1=cmask[:, mo + c0:mo + c1], op=ALU.mult),
                        reads=[ebuf_b[ei], const_b], writes=[pbuf_b[ei]])

                def emit_PV(i, it):
                    c, hh, idx, j, q0, q1 = it
                    ps0, ps1 = hh * 64, (hh + 1) * 64
                    sl = j % KW
                    ei = i % 6
                    acc = AB[hh]
                    c0, c1 = q0 * 128, q1 * 128
                    n = len(jlist)
                    vap = Vw[:, sl, c * 192 + hh * 64:c * 192 + hh * 64 + 128]
                    sch.op("pe", lambda e: e.matmul(
                        bank(acc)[:, c0:c1], lhsT=vap, rhs=pbuf[ei][:, c0:c1], start=(idx == 0), stop=(idx == n - 1)),
                        reads=[kv_b[sl], pbuf_b[ei]], writes=[bankbuf[acc]])
                    if idx == n - 1:
                        f = (c * 2 + hh) % 4
                        d0, d1 = (1 - hh) * 64, (2 - hh) * 64
                        sch.op("dve", lambda e: e.reciprocal(out=ftmp[f][d0:d1, :], in_=bank(acc)[d0:d1, :]),
                               reads=[bankbuf[acc]], writes=[ftmp_b[f]])
                        sch.op("dve", lambda e: e.tensor_tensor(
                            out=oT[ps0:ps1, c, t00:t00 + 512], in0=bank(acc)[ps0:ps1, :], in1=ftmp[f][d0:d1, :], op=ALU.mult),
                            reads=[bankbuf[acc], ftmp_b[f]], writes=[oT_b[c]])

                for i in range(len(items) + LA):
                    if i < len(items):
                        emit_S(i, items[i])
                    if i - LA >= 0:
                        emit_PV(i - LA, items[i - LA])

            out_proj(wout_d, oT, oT_b)

        if "cross" in stages:
            norm_T(2, NS, lambda s: xnT_b[s // 4])
            stage_guard()
            for blk in range(2):
                slot = load_w(wcq_d, 0, 8, blk * 512)
                for g in range(NG):
                    for cc in range(4):
                        bk = nb()
                        c = blk * 4 + cc
                        mm_group(bk, 0, 512, [(wslot[slot][:, kc, cc * 128:(cc + 1) * 128], xnT[:, kc, g * 512:(g + 1) * 512]) for kc in range(NKC)],
                                 [wslot_b[slot], xnT_b[g]])
                        sch.op("act", lambda e, bk=bk, c=c, g=g: e.activation(out=qcT[:, c, g * 512:(g + 1) * 512], in_=bank(bk), func=AF.Copy),
                               reads=[bankbuf[bk]], writes=[qcT_b[c]])
            for g in range(NG):
                toks = slice(g * 512, (g + 1) * 512)

                def emit_CS(h):
                    for m in range(2):
                        sbk = (2 * h + m) % 4
                        ei = (2 * h + m) % 4
                        mm_group(sbk, 0, 512, [(KmT[:, 2 * h + dc, m * 128:(m + 1) * 128], qcT[:, 2 * h + dc, toks]) for dc in range(2)],
                                 [memkv_b, qcT_b[2 * h], qcT_b[2 * h + 1]])
                        sch.op("act", lambda e, ei=ei, sbk=sbk: e.activation(out=pbuf[ei], in_=bank(sbk), func=AF.Exp, scale=1.0 / 16),
                               reads=[bankbuf[sbk]], writes=[pbuf_b[ei]])

                def emit_CU(h):
                    pe_ = [(2 * h) % 4, (2 * h + 1) % 4]
                    db = 4 + (h % 2)
                    mm_group(db, 0, 512, [(ones_b, pbuf[pe_[m]]) for m in range(2)], [const_b, pbuf_b[pe_[0]], pbuf_b[pe_[1]]])
                    f = h % 4
                    sch.op("dve", lambda e, f=f, db=db: e.reciprocal(out=ftmp[f], in_=bank(db)),
                           reads=[bankbuf[db]], writes=[ftmp_b[f]])
                    for dc in range(2):
                        ub = 6 + dc
                        mm_group(ub, 0, 512, [(VmTM[:, m, (2 * h + dc) * 128:(2 * h + dc + 1) * 128], pbuf[pe_[m]]) for m in range(2)],
                                 [memkv_b, pbuf_b[pe_[0]], pbuf_b[pe_[1]]])
                        sch.op("dve", lambda e, f=f, ub=ub, c=2 * h + dc, toks=toks: e.tensor_tensor(out=ocT[:, c, toks], in0=bank(ub), in1=ftmp[f], op=ALU.mult),
                               reads=[bankbuf[ub], ftmp_b[f]], writes=[ocT_b[2 * h + dc]])

                emit_CS(0)
                for h in range(4):
                    if h + 1 < 4:
                        emit_CS(h + 1)
                    emit_CU(h)
            out_proj(wco_d, ocT, ocT_b)

        if "ffn2" in stages:
            norm_T(3, NS, lambda s: xnT_b[s // 4])
            ffn(w2i_d, w2o_d)

        for s in range(NS):
            rstd, rb = rms_stats(x_tm[:, s, :], [x_b[s]])
            k = s % 2
            sch.op("dve", lambda e, s=s, k=k, rstd=rstd: e.scalar_tensor_tensor(
                out=ost[k], in0=x_tm[:, s, :], scalar=rstd, in1=wnf, op0=ALU.mult, op1=ALU.mult),
                reads=[x_b[s], rb, const_b], writes=[ost_b[k]])
            sch.op("sp", lambda e, s=s, k=k, t0=t0: e.dma_start(out=y_d[t0 + s * 128:t0 + (s + 1) * 128, :], in_=ost[k]),
                   reads=[ost_b[k]], dma="ost%d" % k, signal=True)

    print("arena used", apos[0], "of", ARENA)
    sch.emit(final_waits=("ost0", "ost1"))
    return nc


def _consts(S):
    pos = np.arange(S, dtype=np.float32)
    half = 8
    inv = np.exp(-math.log(500000.0) * np.arange(half, dtype=np.float32) / half).astype(np.float32)
    ang = pos[None, :] * inv[:, None]
    cosA = np.ones((64, S), np.float32)
    sinA = np.zeros((64, S), np.float32)
    cosA[0:8] = np.cos(ang)
    cosA[8:16] = np.cos(ang)
    sinA[0:8] = -np.sin(ang)
    sinA[8:16] = np.sin(ang)
    cosA = np.concatenate([cosA, cosA], 0)
    sinA = np.concatenate([sinA, sinA], 0)
    half = 32
    inv = np.exp(-math.log(10000.0) * np.arange(half, dtype=np.float32) / half).astype(np.float32)
    ang = pos[None, :] * inv[:, None]
    cosR = np.concatenate([np.cos(ang), np.cos(ang)], 0)
    sinR = np.concatenate([-np.sin(ang), np.sin(ang)], 0)
    cosR = np.concatenate([cosR, cosR], 0).astype(np.float32)
    sinR = np.concatenate([sinR, sinR], 0).astype(np.float32)
    j = np.arange(128)[:, None, None]
    o = np.arange(-3, 20)[None, :, None]
    i = np.arange(128)[None, None, :]
    dl = 128 * o + i - j
    cm = ((dl >= 0) & (dl <= 128)).astype(np.float32)
    cm += ((dl >= 0) & (dl <= 512) & (dl % 4 == 0))
    cm += ((dl >= 0) & (dl <= 2048) & (dl % 16 == 0))
    cm = cm.reshape(128, 23 * 128).astype(np.float32)
    lg = np.log1p(-(2.0 ** (-5.0 - np.arange(4, dtype=np.float32)))).astype(np.float32)
    idx = np.arange(128, dtype=np.float32)
    diff = idx[None, :] - idx[:, None]
    dT = np.where(diff[None] >= 0, np.exp(lg[:, None, None] * np.maximum(diff, 0.0)[None]), 0.0) * (64 ** -0.5)
    dT = np.transpose(dT, (1, 0, 2)).reshape(128, 4 * 128).astype(np.float32)
    qd = np.exp(lg[:, None] * (idx + 1.0)[None])
    qdec = np.zeros((128, 2, 512), np.float32)
    for pr in range(2):
        for hh in range(2):
            qdec[hh * 64:(hh + 1) * 64, pr, :] = np.tile(qd[2 * pr + hh], 4)[None, :]
    qdec = qdec.reshape(128, 1024)
    small = np.zeros((128, 8), np.float32)
    kd = np.exp(lg[:, None] * (127.0 - idx)[None]) * (64 ** -0.5)
    small[:, 0:4] = kd.T
    cd = np.exp(lg * 128.0)
    for pr in range(2):
        small[0:64, 4 + pr] = cd[2 * pr]
        small[64:128, 4 + pr] = cd[2 * pr + 1]
    ident = np.eye(128, dtype=np.float32)
    return dict(cosA=cosA, sinA=sinA, cosR=cosR, sinR=sinR, cmask=cm, dT=dT, qdec=qdec, small=small, ident=ident)


def _ffn_in_layout(w):
    g = w[:, :DFF].reshape(D, 11, 256)
    u = w[:, DFF:].reshape(D, 11, 256)
    return np.ascontiguousarray(np.concatenate([g, u], axis=2).reshape(D, 2 * DFF))


def _win_layout(w):
    aq, ak, av = w[:, 0:512], w[:, 512:1024], w[:, 1024:1536]
    rq, rk = w[:, 1536:1792], w[:, 1792:2048]
    rv, rg = w[:, 2048:2560], w[:, 2560:3072]

    def partner(a, half):
        n = a.shape[1] // 64
        idx = np.arange(64)
        p = idx.copy()
        p[0:half] = idx[0:half] + half
        p[half:2 * half] = idx[half:2 * half] - half
        cols = (np.arange(n)[:, None] * 64 + p[None, :]).reshape(-1)
        return a[:, cols]

    def inter(a, ap):
        n = a.shape[1] // 128
        return np.concatenate([a.reshape(D, n, 1, 128), ap.reshape(D, n, 1, 128)], axis=2).reshape(D, 2 * a.shape[1])

    parts = [inter(aq, partner(aq, 8)), inter(ak, partner(ak, 8)), inter(rq, partner(rq, 32)),
             inter(rk, partner(rk, 32)), rg, av, rv]
    return np.ascontiguousarray(np.concatenate(parts, axis=1))


def _prep_shared(inp, S):
    f = lambda a: np.ascontiguousarray(np.asarray(a, dtype=np.float32))
    sh = dict(
        w1i=_ffn_in_layout(f(inp["w_ffn1_in"])[0]), w1o=f(inp["w_ffn1_out"])[0],
        w2i=_ffn_in_layout(f(inp["w_ffn2_in"])[0]), w2o=f(inp["w_ffn2_out"])[0],
        win=_win_layout(f(inp["w_in"])[0]), wout=f(inp["w_out"])[0],
        wcq=f(inp["w_cq"])[0], wckv=f(inp["w_ckv"])[0], wco=f(inp["w_co"])[0],
    )
    norms = np.stack([f(inp["norm_ffn1"])[0], f(inp["norm_mix"])[0], f(inp["norm_cross"])[0],
                      f(inp["norm_ffn2"])[0], f(inp["norm_mem"])[0]], 0)
    sh["normsT"] = np.ascontiguousarray(norms.reshape(5, 8, 128).transpose(2, 0, 1).reshape(128, 40))
    sh["wnf"] = np.ascontiguousarray(np.broadcast_to(f(inp["norm_final"])[None, :], (128, D)))
    sh.update(_consts(S))
    return sh


_NC_CACHE = {}


def kernel(**inputs):
    x = np.asarray(inputs["x"], dtype=np.float32)
    mem = np.asarray(inputs["mem"], dtype=np.float32)
    B, S, _ = x.shape
    sh = _prep_shared(inputs, S)
    key = (S,)
    if key not in _NC_CACHE:
        _NC_CACHE[key] = build(S)
    nc = _NC_CACHE[key]
    in_maps = []
    for b in range(B):
        m = dict(sh)
        m["x"] = np.ascontiguousarray(x[b])
        m["mem"] = np.ascontiguousarray(mem[b])
        in_maps.append(m)
    res = run_bass_kernel_spmd(nc, in_maps, core_ids=list(range(B)))
    return np.stack([np.asarray(r["y"], dtype=np.float32) for r in res.results], 0)
```
